# Optimizing a Trainium2 kernel written in Bass

```python
import jax, jax.numpy as jnp
from jax import lax
import numpy as np

D_MODEL = 2048
BATCH = 8
SEQ = 2048
DEPTH = 4

CHUNK = 64
MIX_WIDTH = D_MODEL
GROUP_WIDTH = MIX_WIDTH // 4
CONV_K = 3
SB_HEADS = 4
SB_HEAD_DIM = GROUP_WIDTH // SB_HEADS
SB_BLOCK = 128
GLA_HEADS = 4
GLA_DV = GROUP_WIDTH // GLA_HEADS
GLA_DK = GLA_DV // 2
GLA_GATE_RANK = 16
GLA_TAU = 16.0
POOL_WINDOWS = (2, 4, 8, 16)
POOL_GROUP = GROUP_WIDTH // len(POOL_WINDOWS)
D_FF = -((-8 * D_MODEL) // (3 * 256)) * 256
RMS_EPS = 1e-6

IN_SPLIT_SIZES = (GROUP_WIDTH, GROUP_WIDTH, GROUP_WIDTH,
                  GROUP_WIDTH, GROUP_WIDTH, GROUP_WIDTH,
                  GLA_HEADS * GLA_DK, GLA_HEADS * GLA_DK, GROUP_WIDTH, GROUP_WIDTH, GLA_GATE_RANK,
                  GROUP_WIDTH)
IN_COLS = sum(IN_SPLIT_SIZES)
IN_SPLIT_POINTS = tuple(int(v) for v in np.cumsum(IN_SPLIT_SIZES)[:-1])

kernel_name = 'hybrid_parallel_conv_stickbreak_gla_pool_block'


def rms_norm(x, g):
    xf = x.astype(jnp.float32)
    y = xf * lax.rsqrt(jnp.mean(xf * xf, axis=-1, keepdims=True) + RMS_EPS)
    return (y * g.astype(jnp.float32)).astype(x.dtype)


def short_conv_mixer(b, c, h, conv_w):
    S = h.shape[1]
    u = jnp.pad(c * h, ((0, 0), (CONV_K - 1, 0), (0, 0)))
    y = conv_w[0] * u[:, 0:S]
    for i in range(1, CONV_K):
        y = y + conv_w[i] * u[:, i:i + S]
    return b * y


def stick_breaking_mixer(q, k, v, q_g, k_g):
    Bn, S, _ = q.shape
    def heads(t):
        return t.reshape(Bn, S, SB_HEADS, SB_HEAD_DIM).transpose(0, 2, 1, 3)
    qh = rms_norm(heads(q), q_g).astype(jnp.float32)
    kh = rms_norm(heads(k), k_g).astype(jnp.float32)
    vh = heads(v).astype(jnp.float32)
    scale = SB_HEAD_DIM ** -0.5
    outs = []
    for blk in range(S // SB_BLOCK):
        start = blk * SB_BLOCK
        end = start + SB_BLOCK
        kp = kh[:, :, :end]
        vp = vh[:, :, :end]
        z = jnp.einsum('bhtd,bhsd->bhts', qh[:, :, start:end], kp) * scale
        mask = jnp.arange(end)[None, :] < jnp.arange(start, end)[:, None]
        log_rem = jnp.where(mask, jax.nn.log_sigmoid(-z), 0.0)
        csum = jnp.cumsum(log_rem, axis=-1)
        log_a = jax.nn.log_sigmoid(z) + csum[..., -1:] - csum
        a = jnp.where(mask, jnp.exp(log_a), 0.0)
        outs.append(jnp.einsum('bhts,bhsd->bhtd', a, vp))
    o = jnp.concatenate(outs, axis=2)
    return o.transpose(0, 2, 1, 3).reshape(Bn, S, GROUP_WIDTH).astype(q.dtype)


def gla_mixer(q, k, v, r, a_lr, a_w, a_b, norm_g):
    Bn, S, _ = q.shape
    n_chunks = S // CHUNK
    log_f = jax.nn.log_sigmoid((a_lr @ a_w + a_b).astype(jnp.float32)) / GLA_TAU
    def chunks(t, d):
        t = t.astype(jnp.float32).reshape(Bn, n_chunks, CHUNK, GLA_HEADS, d)
        return t.transpose(1, 0, 3, 2, 4)
    qc = chunks(q * GLA_DK ** -0.5, GLA_DK)
    kc = chunks(k, GLA_DK)
    vc = chunks(v, GLA_DV)
    gc = chunks(log_f, GLA_DK)
    causal = jnp.tril(jnp.ones((CHUNK, CHUNK), dtype=bool))[..., None]

    def step(state, inp):
        qi, ki, vi, gi = inp
        b = jnp.cumsum(gi, axis=-2)
        o_inter = jnp.einsum('bhtk,bhkv->bhtv', qi * jnp.exp(b), state)
        diff = b[:, :, :, None, :] - b[:, :, None, :, :]
        decay = jnp.where(causal, jnp.exp(jnp.where(causal, diff, 0.0)), 0.0)
        att = jnp.einsum('bhtk,bhsk,bhtsk->bhts', qi, ki, decay)
        o = o_inter + jnp.einsum('bhts,bhsv->bhtv', att, vi)
        b_last = b[:, :, -1:, :]
        state = (jnp.exp(b_last[:, :, 0, :, None]) * state
                 + jnp.einsum('bhsk,bhsv->bhkv', ki * jnp.exp(b_last - b), vi))
        return state, o

    s0 = jnp.zeros((Bn, GLA_HEADS, GLA_DK, GLA_DV), jnp.float32)
    _, o = lax.scan(step, s0, (qc, kc, vc, gc))
    o = o.transpose(1, 0, 3, 2, 4).reshape(Bn, S, GLA_HEADS, GLA_DV)
    o = rms_norm(o, norm_g).reshape(Bn, S, GROUP_WIDTH)
    o = o * jax.nn.silu(r.astype(jnp.float32))
    return o.astype(q.dtype)


def pool_mixer(u, pool_w, pool_scale):
    Bn, S, _ = u.shape
    uf = u.astype(jnp.float32).reshape(Bn, S, len(POOL_WINDOWS), POOL_GROUP)
    cs = jnp.cumsum(uf, axis=1)
    outs = []
    for g, w in enumerate(POOL_WINDOWS):
        csg = cs[:, :, g]
        prev = jnp.pad(csg[:, :S - w], ((0, 0), (w, 0), (0, 0)))
        count = jnp.minimum(jnp.arange(1, S + 1), w).astype(jnp.float32)[None, :, None]
        outs.append((csg - prev) / count - uf[:, :, g])
    pooled = jnp.stack(outs, axis=2)
    y = jnp.einsum('bsgc,gcd->bsgd', pooled, pool_w.astype(jnp.float32)).reshape(Bn, S, GROUP_WIDTH)
    return (y * pool_scale.astype(jnp.float32)).astype(u.dtype)


def setup_inputs(seed: int = 0) -> dict:
    key = jax.random.key(seed)
    ks = jax.random.split(key, 16)
    f32 = jnp.float32
    def nrm(k, shape, scale):
        return jax.random.normal(k, shape, f32) * scale
    return {
        'x': jax.random.normal(ks[0], (BATCH, SEQ, D_MODEL), f32),
        'norm1_g': 1.0 + nrm(ks[1], (DEPTH, D_MODEL), 0.02),
        'w_in': nrm(ks[2], (DEPTH, D_MODEL, IN_COLS), D_MODEL ** -0.5),
        'conv_w': nrm(ks[3], (DEPTH, CONV_K, GROUP_WIDTH), CONV_K ** -0.5),
        'sb_q_g': 1.0 + nrm(ks[4], (DEPTH, SB_HEAD_DIM), 0.02),
        'sb_k_g': 1.0 + nrm(ks[5], (DEPTH, SB_HEAD_DIM), 0.02),
        'gla_a_w': nrm(ks[6], (DEPTH, GLA_GATE_RANK, GLA_HEADS * GLA_DK), GLA_GATE_RANK ** -0.5),
        'gla_a_b': nrm(ks[7], (DEPTH, GLA_HEADS * GLA_DK), 0.1),
        'gla_norm_g': 1.0 + nrm(ks[8], (DEPTH, GLA_DV), 0.02),
        'pool_w': nrm(ks[9], (DEPTH, len(POOL_WINDOWS), POOL_GROUP, POOL_GROUP), POOL_GROUP ** -0.5),
        'pool_scale': 1.0 + nrm(ks[10], (DEPTH, GROUP_WIDTH), 0.02),
        'w_out': nrm(ks[11], (DEPTH, MIX_WIDTH, D_MODEL), MIX_WIDTH ** -0.5),
        'norm2_g': 1.0 + nrm(ks[12], (DEPTH, D_MODEL), 0.02),
        'w_gate': nrm(ks[13], (DEPTH, D_MODEL, D_FF), D_MODEL ** -0.5),
        'w_up': nrm(ks[14], (DEPTH, D_MODEL, D_FF), D_MODEL ** -0.5),
        'w_down': nrm(ks[15], (DEPTH, D_FF, D_MODEL), D_FF ** -0.5),
    }


def reference(x, norm1_g, w_in, conv_w, sb_q_g, sb_k_g, gla_a_w, gla_a_b, gla_norm_g,
              pool_w, pool_scale, w_out, norm2_g, w_gate, w_up, w_down):
    for l in range(DEPTH):
        h = rms_norm(x, norm1_g[l])
        proj = h @ w_in[l]
        (c_b, c_c, c_h, s_q, s_k, s_v,
         g_q, g_k, g_v, g_r, g_a, p_u) = jnp.split(proj, IN_SPLIT_POINTS, axis=-1)
        y_conv = short_conv_mixer(c_b, c_c, c_h, conv_w[l])
        y_sb = stick_breaking_mixer(s_q, s_k, s_v, sb_q_g[l], sb_k_g[l])
        y_gla = gla_mixer(g_q, g_k, g_v, g_r, g_a, gla_a_w[l], gla_a_b[l], gla_norm_g[l])
        y_pool = pool_mixer(p_u, pool_w[l], pool_scale[l])
        mixed = jnp.concatenate([y_conv, y_sb, y_gla, y_pool], axis=-1)
        x = x + mixed @ w_out[l]
        h = rms_norm(x, norm2_g[l])
        x = x + (jax.nn.silu(h @ w_gate[l]) * (h @ w_up[l])) @ w_down[l]
    return x
```

```python
import contextlib
from collections import deque

import numpy as np
import concourse.bass as bass
import concourse.mybir as mybir
from concourse.bass_utils import run_bass_kernel_spmd

F32 = mybir.dt.float32
BF16 = mybir.dt.bfloat16
AF = mybir.ActivationFunctionType
ALU = mybir.AluOpType

D = 2048
S = 2048
L = 4
NCORES = 8
INC = 5136
DFF = 5632
NFF = DFF // 128
TT = 512
NTILE = S // TT
EPS = 1e-6
NW = 4
SQRT128 = float(np.sqrt(128.0))
import os
SAME_ENGINE_SYNC = os.environ.get("K_SES", "1") == "1"

C_CONV = 0
C_SQ = 1536
C_SK = 2048
C_SV = 2560
C_GQ = 3072
C_GK = 3328
C_GV = 3584
C_GR = 4096
C_GA = 4608
C_PU = 4624

PP_CONV = 0
PP_SBQ = 12
PP_SBK = 13
PP_GLN = 14
PP_PSC = 15
NPP = 19

K_IDENT = 0
K_UINC = 128
K_TRI = 256
K_SBM = 384
K_PRC = 384 + 128
NCST = K_PRC + 64


ENGS = ("pe", "act", "dve", "pool", "sp")


class Op:
    __slots__ = ("eng", "fn", "deps", "dma", "needs_inc", "count", "semkey", "idx", "waits")


class Sched:
    def __init__(self):
        self.ops = {e: [] for e in ENGS}
        self.lastw = {}
        self.readers = {}
        self.last_by_key = {}

    def add(self, eng, fn, reads=(), writes=(), dma=None):
        op = Op()
        op.eng = eng
        op.fn = fn
        op.dma = dma
        op.semkey = ("dma", dma) if dma is not None else ("eng", eng)
        op.needs_inc = dma is not None
        op.count = None
        op.waits = None
        deps = set()
        for r in reads:
            w = self.lastw.get(r)
            if w is not None:
                deps.add(w)
        for wkey in writes:
            w = self.lastw.get(wkey)
            if w is not None:
                deps.add(w)
            rd = self.readers.get(wkey)
            if rd:
                deps.update(rd.values())
        op.deps = deps
        for r in reads:
            self.readers.setdefault(r, {})[op.semkey] = op
        for wkey in writes:
            self.lastw[wkey] = op
            self.readers[wkey] = {}
        op.idx = len(self.ops[eng])
        self.ops[eng].append(op)
        if fn is not None and dma is None:
            self.last_by_key[op.semkey] = op
        return op

    def fence(self):
        lasts = [self.last_by_key[("eng", e)] for e in ("pe", "act", "dve") if ("eng", e) in self.last_by_key]
        for e in ("pe", "act", "dve"):
            op = self.add(e, None)
            op.deps = set(lasts)

    def finalize(self):
        for e in ENGS:
            known = {}
            for op in self.ops[e]:
                need = {}
                for d in op.deps:
                    if d.dma is None and d.eng == e:
                        if e == "pe" or not SAME_ENGINE_SYNC:
                            continue
                    k = d.semkey
                    if known.get(k, -1) >= d.idx:
                        continue
                    if k not in need or need[k].idx < d.idx:
                        need[k] = d
                for k, d in need.items():
                    known[k] = d.idx
                    d.needs_inc = True
                op.waits = list(need.values())
        counters = {}
        for e in ENGS:
            for op in self.ops[e]:
                if op.needs_inc and op.fn is not None:
                    k = op.semkey
                    counters[k] = counters.get(k, 0) + (16 if op.dma is not None else 1)
                    op.count = counters[k]
        return counters


def build_program(n_layers=L, dbg=None, dbg_tile=0):
    nc = bass.Bass("TRN2", target_bir_lowering=False)
    dbg = dbg or {}

    def din(name, shape):
        return nc.dram_tensor(name, list(shape), F32, kind="ExternalInput").ap()

    x_in = din("x", (S, D))
    w_in = din("w_in", (L, D, INC))
    w_out = din("w_out", (L, D, D))
    w_gate = din("w_gate", (L, D, DFF))
    w_up = din("w_up", (L, D, DFF))
    w_down = din("w_down", (L, DFF, D))
    g1b_d = din("g1b", (L, 128, D))
    g2b_d = din("g2b", (L, 128, D))
    pp_d = din("pp", (L, 128, NPP))
    aw_d = din("aw", (L, 16, 256))
    ab_d = din("ab", (L, 1, 256))
    pw_d = din("pool_w", (L, 4, 128, 128))
    cst_d = din("cst", (128, NCST))
    y_out = nc.dram_tensor("y", [S, D], F32, kind="ExternalOutput").ap()
    xs = nc.dram_tensor("xs", [S, D], F32, kind="Internal").ap()
    dbg_out = {}
    for name, shape in dbg.items():
        dbg_out[name] = nc.dram_tensor("dbg_" + name, list(shape), F32, kind="ExternalOutput").ap()

    Sd = Sched()
    es = contextlib.ExitStack()

    def sb(name, shape, dt):
        return es.enter_context(nc.sbuf_tensor("sb_" + name, list(shape), dt))

    cst = sb("cst", (128, NCST), F32)
    identb = sb("identb", (128, 128), BF16)
    onesb = sb("onesb", (128, 128), BF16)
    onesf = sb("onesf", (128, 128), F32)
    uincb = sb("uincb", (128, 128), BF16)
    gbuf = [sb(f"gb{i}", (128, D), F32) for i in range(2)]
    pp = sb("pp", (128, NPP), F32)
    aw = sb("aw", (16, 256), F32)
    ab = sb("ab", (1, 256), F32)
    pwb = sb("pwb", (128, 4, 128), BF16)
    hT = sb("hT", (128, 16, TT), BF16)
    wslots = [sb(f"wslot{i}", (128, 16 * 256), BF16) for i in range(NW)]
    xring = [sb(f"xring{i}", (128, D), F32) for i in range(2)]
    hbring = [sb(f"hbring{i}", (128, D), BF16) for i in range(2)]
    stat = sb("stat", (128, 64), F32)
    NXO = 4
    xo = [sb(f"xo{i}", (128, 512), F32) for i in range(NXO)]
    ccar = sb("ccar", (128, 4, 2), F32)
    pcar = sb("pcar", (128, 4, 16), F32)
    dbgbuf = [sb(f"dbgbuf{i}", (128, 512), F32) for i in range(1)] if dbg else None

    UN = 51840
    U = sb("U", (128, UN), BF16)
    ucur = [0]

    def carve(nelem, dt):
        n16 = nelem * (2 if dt == F32 else 1)
        a = ucur[0]
        assert a + n16 <= UN, f"union overflow {a + n16} > {UN}"
        ucur[0] = a + n16
        v = U[:, a:a + n16]
        if dt == F32:
            v = v.bitcast(F32)
        return v

    banks = [es.enter_context(nc.psum_tensor(f"bank{i}", [128, 512], F32)) for i in range(8)]
    freeb = deque(range(8))

    def getbank():
        return freeb.popleft()

    def relbank(b):
        freeb.append(b)

    def BK(b):
        return ("B", b)

    def A(eng, fn, reads=(), writes=(), dma=None):
        return Sd.add(eng, fn, reads, writes, dma)

    def mm(out, lhsT, rhs, start, stop):
        return lambda e: e.matmul(out, lhsT, rhs, start=start, stop=stop)

    def act(out, in_, func, **kw):
        return lambda e: e.activation(out=out, in_=in_, func=func, **kw)

    def tcopy(out, in_):
        return lambda e: e.tensor_copy(out=out, in_=in_)

    def tt(out, in0, in1, op):
        return lambda e: e.tensor_tensor(out=out, in0=in0, in1=in1, op=op)

    def ts(out, in0, s1, s2, op0, op1=None):
        if op1 is None:
            return lambda e: e.tensor_scalar(out=out, in0=in0, scalar1=s1, scalar2=None, op0=op0)
        return lambda e: e.tensor_scalar(out=out, in0=in0, scalar1=s1, scalar2=s2, op0=op0, op1=op1)

    def stt(out, in0, scalar, in1, op0, op1):
        return lambda e: e.scalar_tensor_tensor(out=out, in0=in0, scalar=scalar, in1=in1, op0=op0, op1=op1)

    def dma(out, in_):
        return lambda e: e.dma_start(out=out, in_=in_)

    def memset(ap, v):
        return lambda e: e.memset(ap, v)

    dbg_n = [0]

    def dump(name, src_ap, reads, rows=None):
        if name not in dbg_out:
            return
        dst = dbg_out[name]
        dbg_n[0] += 1
        A("sp", dma(dst if rows is None else dst[rows], src_ap), reads=reads, writes=[("dbg", name, dbg_n[0])],
          dma=f"dbg{dbg_n[0] % 4}")

    stat_i = [0]

    def statcol():
        c = stat_i[0] % 64
        stat_i[0] += 1
        return c

    A("sp", dma(cst[:], cst_d[:, :]), writes=["cst"], dma="c0")
    A("dve", tcopy(identb[:], cst[:, K_IDENT:K_IDENT + 128]), reads=["cst"], writes=["identb"])
    A("dve", tcopy(uincb[:], cst[:, K_UINC:K_UINC + 128]), reads=["cst"], writes=["uincb"])
    A("dve", memset(onesb[:], 1.0), writes=["onesb"])
    A("dve", memset(onesf[:], 1.0), writes=["onesf"])
    A("dve", memset(stat[:], 12345.0), writes=[("stat", c) for c in range(64)])

    wn = [0]

    def wload(src3, nk, ncol):
        s = wn[0] % NW
        wn[0] += 1
        view = wslots[s][:, 0:nk * ncol].rearrange("p (k n) -> p k n", k=nk)
        A("pool", dma(view, src3), writes=[("w", s)], dma=f"w{s}")
        return view, ("w", s)

    def wsrc(wl, r0, nk, c0, ncol):
        return wl[r0:r0 + nk * 128, c0:c0 + ncol].rearrange("(k p) n -> p k n", p=128)

    HT_ALL = [("hT", tb) for tb in range(4)]

    def fm_group(wt, wkey, off, M, nk=16, rhs_of=None, rhs_keys=None):
        b = getbank()
        for kc in range(nk):
            rhs = hT[:, kc, :] if rhs_of is None else rhs_of(kc)
            A("pe", mm(banks[b][0:M, :], wt[:, kc, off:off + M], rhs, kc == 0, kc == nk - 1),
              reads=[wkey] + (HT_ALL if rhs_keys is None else rhs_keys), writes=[BK(b)])
        return b

    def tm_group(wt, wkey, tb, ncol):
        b = getbank()
        for kc in range(16):
            A("pe", mm(banks[b][:, 0:ncol], hT[:, kc, tb * 128:(tb + 1) * 128], wt[:, kc, 0:ncol], kc == 0, kc == 15),
              reads=[wkey, ("hT", tb)], writes=[BK(b)])
        return b

    xn = [0]
    evac_flip = [0]

    norm_slot = {}

    def norm_pre(src, srcname, ti, tb, gb, gbkey):
        r = ti * 4 + tb
        sl = xn[0] % 2
        xn[0] += 1
        norm_slot[tb] = sl
        xk = ("xring", sl)
        hk = ("hbring", sl)
        A("sp", dma(xring[sl][:], src[r * 128:(r + 1) * 128, :]),
          reads=[(srcname, r, n) for n in range(4)], writes=[xk], dma=f"xl{sl}")
        c = statcol()
        A("act", act(hbring[sl][:], xring[sl][:], AF.Square, accum_out=stat[:, c:c + 1]),
          reads=[xk], writes=[hk, ("stat", c)])
        A("act", act(stat[:, c:c + 1], stat[:, c:c + 1], AF.Ln, scale=1.0 / D, bias=EPS),
          reads=[("stat", c)], writes=[("stat", c)])
        A("act", act(stat[:, c:c + 1], stat[:, c:c + 1], AF.Exp, scale=-0.5),
          reads=[("stat", c)], writes=[("stat", c)])
        A("dve", stt(hbring[sl][:], xring[sl][:], stat[:, c:c + 1], gb[:], ALU.mult, ALU.mult),
          reads=[xk, ("stat", c), gbkey], writes=[hk])

    def norm_post(tb):
        sl = norm_slot[tb]
        hk = ("hbring", sl)
        for half in range(2):
            b = getbank()
            bv = banks[b][:].bitcast(BF16)
            for k8 in range(8):
                kc = half * 8 + k8
                A("pe", lambda e, o=bv[:, k8 * 128:(k8 + 1) * 128], i=hbring[sl][:, kc * 128:(kc + 1) * 128]:
                  e.transpose(out=o, in_=i, identity=identb[:]),
                  reads=[hk, "identb"], writes=[BK(b)])
            src_v = bv[:, 0:1024].rearrange("p (k t) -> p k t", k=8)
            dst_v = hT[:, half * 8:(half + 1) * 8, tb * 128:(tb + 1) * 128]
            eng = "act" if (evac_flip[0] % 2 == 0) else "dve"
            evac_flip[0] += 1
            if eng == "act":
                A("act", act(dst_v, src_v, AF.Copy), reads=[BK(b)], writes=[("hT", tb)])
            else:
                A("dve", tcopy(dst_v, src_v), reads=[BK(b)], writes=[("hT", tb)])
            relbank(b)

    xon = [0]

    def out_proj(ti, wl, nkc, actT, actkeys_of, src, srcname, dst, dstname, pre=None, post=None):
        ktiles = [(k0, min(8, nkc - k0)) for k0 in range(0, nkc, 8)]
        for ng in range(4):
            if pre is not None:
                pre(ng)
            bs = [getbank() for _ in range(4)]
            for (k0, nk) in ktiles:
                wt, wkey = wload(wsrc(wl, k0 * 128, nk, ng * 512, 512), nk, 512)
                for tb in range(4):
                    for k8 in range(nk):
                        kc = k0 + k8
                        A("pe", mm(banks[bs[tb]][:, :], actT[:, kc, tb * 128:(tb + 1) * 128], wt[:, k8, :],
                                   kc == 0, kc == nkc - 1),
                          reads=[wkey] + actkeys_of(kc), writes=[BK(bs[tb])])
            if post is not None and ng > 0:
                post(ng - 1)
            for tb in range(4):
                r = ti * 4 + tb
                s = xon[0] % NXO
                xon[0] += 1
                A("sp", dma(xo[s][:], src[r * 128:(r + 1) * 128, ng * 512:(ng + 1) * 512]),
                  reads=[(srcname, r, ng)], writes=[("xo", s)], dma=f"xo{s}")
                A("dve", tt(xo[s][:], xo[s][:], banks[bs[tb]][:, :], ALU.add),
                  reads=[("xo", s), BK(bs[tb])], writes=[("xo", s)])
                A("sp", dma(dst[r * 128:(r + 1) * 128, ng * 512:(ng + 1) * 512], xo[s][:]),
                  reads=[("xo", s)], writes=[(dstname, r, ng)], dma=f"xs{s}")
                relbank(bs[tb])
        if post is not None:
            post(3)

    ucur[0] = 0
    mixedT = carve(16 * TT, BF16).rearrange("p (c t) -> p c t", c=16)
    kTc = carve(4 * S, BF16).rearrange("p (h t) -> p h t", h=4)
    Vc = carve(16 * 512, BF16).rearrange("p (b c) -> p b c", b=16)
    gstate = carve(2 * 256, F32).rearrange("p (c v) -> p c v", c=2)
    gstate_b = carve(2 * 256, BF16).rearrange("p (c v) -> p c v", c=2)
    a_mix_base = ucur[0]
    ubuf = carve(4 * 514, F32).rearrange("p (c t) -> p c t", c=4)
    bbuf = carve(4 * 512, F32).rearrange("p (c t) -> p c t", c=4)
    cbuf = carve(4 * 512, F32).rearrange("p (c t) -> p c t", c=4)
    ytmp = [carve(512, F32) for _ in range(2)]
    conv_end = ucur[0]
    ucur[0] = a_mix_base
    ubp = carve(4 * 528, F32).rearrange("p (c t) -> p c t", c=4)
    plev = [carve(528, F32) for _ in range(4)]
    pooled_f = [carve(512, F32) for _ in range(2)]
    pooled_b = [carve(512, BF16) for _ in range(2)]
    pool_end = ucur[0]
    ucur[0] = a_mix_base
    qT = carve(4 * 512, BF16).rearrange("p (h t) -> p h t", h=4)
    qf = [carve(512, F32) for _ in range(2)]
    sqb = [carve(512, BF16) for _ in range(2)]
    rsb = [carve(512, F32) for _ in range(2)]
    NEB = 3
    Eb = [carve(512, F32) for _ in range(NEB)]
    spb = [carve(512, BF16) for _ in range(NEB)]
    cumb = [carve(512, F32) for _ in range(2)]
    Xb = [carve(512, F32) for _ in range(2)]
    Ab = [carve(512, BF16) for _ in range(NEB)]
    totb = carve(512, F32)
    sb_end = ucur[0]
    ucur[0] = a_mix_base
    gqT = carve(2 * 512, F32).rearrange("p (c t) -> p c t", c=2)
    gkT = carve(2 * 512, F32).rearrange("p (c t) -> p c t", c=2)
    gktm = carve(4 * 256, F32).rearrange("p (b c) -> p b c", b=4)
    gv = carve(4 * 512, BF16).rearrange("p (b c) -> p b c", b=4)
    srT = carve(4 * 512, F32).rearrange("p (c t) -> p c t", c=4)
    alr = carve(512, F32)
    oT = carve(4 * 512, F32).rearrange("p (h t) -> p h t", h=4)
    g_e1 = carve(256, F32)
    g_gp = carve(256, F32)
    g_eb = carve(256, F32).rearrange("p (c t) -> p c t", c=2)
    g_enb = carve(256, F32).rearrange("p (c t) -> p c t", c=2)
    g_enbtm = carve(256, F32)
    g_qe = [carve(256, BF16).rearrange("p (c t) -> p c t", c=2) for _ in range(2)]
    g_ke = [carve(256, BF16).rearrange("p (c t) -> p c t", c=2) for _ in range(2)]
    g_ketm = [carve(256, BF16) for _ in range(2)]
    g_att = [carve(128, BF16) for _ in range(4)]
    g_tmp = carve(256, F32)
    g_sq = [carve(512, BF16) for _ in range(2)]
    g_rn = [carve(512, F32) for _ in range(2)]
    gla_end = ucur[0]
    a_end = max(conv_end, pool_end, sb_end, gla_end)
    ucur[0] = 0
    actT = carve(NFF * TT, BF16).rearrange("p (c t) -> p c t", c=NFF)
    sgb = [carve(512, F32) for _ in range(4)]
    b_end = ucur[0]
    assert max(a_end, b_end) <= UN

    w_tri = cst[:, K_TRI:K_TRI + 128]

    def sweep_a(l, ti, src, srcname, dst, dstname, pre, post):
        t0 = ti * TT
        wl = w_in[l]

        if ti == 0:
            A("dve", memset(ccar[:], 0.0), writes=["ccar"])
        A("dve", tcopy(ubuf[:, :, 0:2], ccar[:]), reads=["ccar"], writes=[("ubuf", c) for c in range(4)])
        yi = 0
        for t in range(6):
            wt, wkey = wload(wsrc(wl, 0, 16, C_CONV + t * 256, 256), 16, 256)
            for j in range(2):
                c = (t % 2) * 2 + j
                b = fm_group(wt, wkey, j * 128, 128)
                if t < 2:
                    A("act", act(bbuf[:, c, :], banks[b][:, :], AF.Copy), reads=[BK(b)], writes=[("bbuf", c)])
                elif t < 4:
                    A("act", act(cbuf[:, c, :], banks[b][:, :], AF.Copy), reads=[BK(b)], writes=[("cbuf", c)])
                else:
                    yt = ytmp[yi % 2]
                    yk = ("ytmp", yi % 2)
                    yi += 1
                    A("dve", tt(ubuf[:, c, 2:514], cbuf[:, c, :], banks[b][:, :], ALU.mult),
                      reads=[("cbuf", c), BK(b)], writes=[("ubuf", c)])
                    A("dve", ts(yt, ubuf[:, c, 2:514], pp[:, PP_CONV + c * 3 + 2:PP_CONV + c * 3 + 3], None, ALU.mult),
                      reads=[("ubuf", c), "pp"], writes=[yk])
                    A("dve", stt(yt, ubuf[:, c, 1:513], pp[:, PP_CONV + c * 3 + 1:PP_CONV + c * 3 + 2], yt,
                                 ALU.mult, ALU.add), reads=[("ubuf", c), "pp", yk], writes=[yk])
                    A("dve", stt(yt, ubuf[:, c, 0:512], pp[:, PP_CONV + c * 3 + 0:PP_CONV + c * 3 + 1], yt,
                                 ALU.mult, ALU.add), reads=[("ubuf", c), "pp", yk], writes=[yk])
                    A("dve", tt(mixedT[:, c, :], yt, bbuf[:, c, :], ALU.mult),
                      reads=[yk, ("bbuf", c)], writes=[("mixedT", c)])
                    A("dve", tcopy(ccar[:, c, :], ubuf[:, c, 512:514]), reads=[("ubuf", c)], writes=["ccar"])
                relbank(b)
        Sd.fence()

        if ti == 0:
            A("dve", memset(pcar[:], 0.0), writes=["pcar"])
        A("dve", tcopy(ubp[:, :, 0:16], pcar[:]), reads=["pcar"], writes=[("ubp", g) for g in range(4)])
        for t in range(2):
            wt, wkey = wload(wsrc(wl, 0, 16, C_PU + t * 256, 256), 16, 256)
            for j in range(2):
                g = t * 2 + j
                win = 2 << g
                b = fm_group(wt, wkey, j * 128, 128)
                A("act", act(ubp[:, g, 16:528], banks[b][:, :], AF.Copy), reads=[BK(b)], writes=[("ubp", g)])
                relbank(b)
                prev = ubp[:, g, :]
                prevk = ("ubp", g)
                sh = 1
                lo = 1
                for lev in range(g + 1):
                    cur = plev[lev]
                    A("dve", tt(cur[:, lo:528], prev[:, lo:528], prev[:, lo - sh:528 - sh], ALU.add),
                      reads=[prevk], writes=[("plev", lev)])
                    prev = cur
                    prevk = ("plev", lev)
                    sh *= 2
                    lo += sh
                pf = pooled_f[g % 2]
                pb_ = pooled_b[g % 2]
                A("dve", stt(pf, prev[:, 16:528], 1.0 / win, ubp[:, g, 16:528], ALU.mult, ALU.subtract),
                  reads=[prevk, ("ubp", g)], writes=[("pooled_f", g % 2)])
                if ti == 0:
                    A("dve", tt(pf[:, 0:16], prev[:, 16:32], cst[:, K_PRC + g * 16:K_PRC + (g + 1) * 16], ALU.mult),
                      reads=[prevk, "cst", ("pooled_f", g % 2)], writes=[("pooled_f", g % 2)])
                    A("dve", tt(pf[:, 0:16], pf[:, 0:16], ubp[:, g, 16:32], ALU.subtract),
                      reads=[("ubp", g), ("pooled_f", g % 2)], writes=[("pooled_f", g % 2)])
                A("dve", tcopy(pb_, pf), reads=[("pooled_f", g % 2)], writes=[("pooled_b", g % 2)])
                A("dve", tcopy(pcar[:, g, :], ubp[:, g, 512:528]), reads=[("ubp", g)], writes=["pcar"])
                b2 = getbank()
                A("pe", mm(banks[b2][:, :], pwb[:, g, :], pb_, True, True),
                  reads=["pwb", ("pooled_b", g % 2)], writes=[BK(b2)])
                A("act", act(mixedT[:, 12 + g, :], banks[b2][:, :], AF.Copy, scale=pp[:, PP_PSC + g:PP_PSC + g + 1]),
                  reads=[BK(b2), "pp"], writes=[("mixedT", 12 + g)])
                relbank(b2)
        Sd.fence()

        qi = 0
        for t in range(4):
            wt, wkey = wload(wsrc(wl, 0, 16, C_SQ + t * 256, 256), 16, 256)
            for j in range(2):
                h = (t % 2) * 2 + j
                isq = t < 2
                b = fm_group(wt, wkey, j * 128, 128)
                s2 = qi % 2
                qi += 1
                A("act", act(qf[s2], banks[b][:, :], AF.Copy), reads=[BK(b)], writes=[("qf", s2)])
                A("act", act(sqb[s2], banks[b][:, :], AF.Square), reads=[BK(b)], writes=[("sqb", s2)])
                relbank(b)
                b2 = getbank()
                A("pe", mm(banks[b2][:, :], onesb[:], sqb[s2], True, True), reads=["onesb", ("sqb", s2)], writes=[BK(b2)])
                A("act", act(rsb[s2], banks[b2][:, :], AF.Ln, bias=128.0 * EPS), reads=[BK(b2)], writes=[("rsb", s2)])
                relbank(b2)
                A("act", act(rsb[s2], rsb[s2], AF.Exp, scale=-0.5), reads=[("rsb", s2)], writes=[("rsb", s2)])
                gcol = PP_SBQ if isq else PP_SBK
                if isq:
                    dstv = qT[:, h, :]
                    dk = ("qT", h)
                else:
                    dstv = kTc[:, h, t0:t0 + TT]
                    dk = ("kT", h, ti)
                A("dve", stt(dstv, qf[s2], pp[:, gcol:gcol + 1], rsb[s2], ALU.mult, ALU.mult),
                  reads=[("qf", s2), "pp", ("rsb", s2)], writes=[dk])
        for t in range(2):
            wt, wkey = wload(wsrc(wl, 0, 16, C_SV + t * 256, 256), 16, 256)
            for tb in range(4):
                b = tm_group(wt, wkey, tb, 256)
                A("act", act(Vc[:, ti * 4 + tb, t * 256:(t + 1) * 256], banks[b][:, 0:256], AF.Copy),
                  reads=[BK(b)], writes=[("Vc", ti * 4 + tb, t)])
                relbank(b)
        pairs = [(h, kb) for h in range(4) for kb in range(ti * 4 + 3, -1, -1)]
        NP = len(pairs)
        st = {}
        po_bank = {}

        def stage_a(n):
            h, kb = pairs[n]
            j = kb - ti * 4
            q0 = max(j, 0) * 128
            e = n % NEB
            bz = getbank()
            A("pe", mm(banks[bz][:, q0:512], kTc[:, h, kb * 128:(kb + 1) * 128], qT[:, h, q0:512], True, True),
              reads=[("kT", h, kb // 4), ("qT", h)], writes=[BK(bz)])
            A("act", act(Eb[e][:, q0:512], banks[bz][:, q0:512], AF.Exp, scale=SQRT128), reads=[BK(bz)], writes=[("E", e)])
            relbank(bz)
            if j >= 0:
                A("dve", tt(Eb[e][:, q0:q0 + 128], Eb[e][:, q0:q0 + 128], cst[:, K_SBM:K_SBM + 128], ALU.mult),
                  reads=[("E", e), "cst"], writes=[("E", e)])
            A("act", act(spb[e][:, q0:512], Eb[e][:, q0:512], AF.Ln, bias=1.0), reads=[("E", e)], writes=[("sp", e)])

        def stage_b(n):
            h, kb = pairs[n]
            j = kb - ti * 4
            q0 = max(j, 0) * 128
            e = n % NEB
            first = kb == ti * 4 + 3
            last = kb == 0
            if first:
                A("dve", memset(totb, 0.0), writes=["tot"])
            bc = getbank()
            A("pe", mm(banks[bc][:, q0:512], uincb[:], spb[e][:, q0:512], True, True), reads=["uincb", ("sp", e)], writes=[BK(bc)])
            c2 = n % 2
            A("dve", tt(cumb[c2][:, q0:512], banks[bc][:, q0:512], totb[:, q0:512], ALU.add),
              reads=[BK(bc), "tot"], writes=[("cum", c2)])
            relbank(bc)
            if not last:
                bo = getbank()
                A("pe", mm(banks[bo][:, q0:512], onesb[:], spb[e][:, q0:512], True, True), reads=["onesb", ("sp", e)], writes=[BK(bo)])
                A("dve", tt(totb[:, q0:512], totb[:, q0:512], banks[bo][:, q0:512], ALU.add), reads=[BK(bo), "tot"], writes=["tot"])
                relbank(bo)
            A("act", act(Xb[c2][:, q0:512], cumb[c2][:, q0:512], AF.Exp, scale=-1.0), reads=[("cum", c2)], writes=[("X", c2)])
            if q0 > 0:
                A("pool", memset(Ab[e][:, 0:q0], 0.0), writes=[("A", e)])
            A("pool", tt(Ab[e][:, q0:512], Eb[e][:, q0:512], Xb[c2][:, q0:512], ALU.mult), reads=[("E", e), ("X", c2)], writes=[("A", e)])

        def stage_c(n):
            h, kb = pairs[n]
            e = n % NEB
            first = kb == ti * 4 + 3
            last = kb == 0
            if first:
                po_bank[h] = getbank()
            bp = po_bank[h]
            A("pe", mm(banks[bp][:, :], Vc[:, kb, h * 128:(h + 1) * 128], Ab[e], first, last),
              reads=[("Vc", kb, h // 2), ("A", e)], writes=[BK(bp)])
            if last:
                A("act", act(mixedT[:, 4 + h, :], banks[bp][:, :], AF.Copy), reads=[BK(bp)], writes=[("mixedT", 4 + h)])
                relbank(bp)

        gla_pref = [wload(wsrc(wl, 0, 16, C_GQ, 256), 16, 256),
                    wload(wsrc(wl, 0, 16, C_GK, 256), 16, 256),
                    wload(wsrc(wl, 0, 16, C_GV, 256), 16, 256)]
        for n in range(NP + 2):
            if n < NP:
                stage_a(n)
            if 0 <= n - 1 < NP:
                stage_b(n - 1)
            if 0 <= n - 2 < NP:
                stage_c(n - 2)
        Sd.fence()

        if ti == 0:
            A("dve", memset(gstate[:], 0.0), writes=[("gstate", c) for c in range(2)])
            A("dve", memset(gstate_b[:], 0.0), writes=[("gstate_b", c) for c in range(2)])
        wt, wkey = gla_pref[0]
        for c in range(2):
            b = fm_group(wt, wkey, c * 128, 128)
            A("act", act(gqT[:, c, :], banks[b][:, :], AF.Copy), reads=[BK(b)], writes=[("gqT", c)])
            relbank(b)
        wt, wkey = gla_pref[1]
        for c in range(2):
            b = fm_group(wt, wkey, c * 128, 128)
            A("act", act(gkT[:, c, :], banks[b][:, :], AF.Copy), reads=[BK(b)], writes=[("gkT", c)])
            relbank(b)
        for tb in range(4):
            b = tm_group(wt, wkey, tb, 256)
            A("act", act(gktm[:, tb, :], banks[b][:, 0:256], AF.Copy), reads=[BK(b)], writes=[("gktm", tb)])
            relbank(b)
        for t in range(2):
            wt, wkey = gla_pref[2] if t == 0 else wload(wsrc(wl, 0, 16, C_GV + t * 256, 256), 16, 256)
            for tb in range(4):
                b = tm_group(wt, wkey, tb, 256)
                A("act", act(gv[:, tb, t * 256:(t + 1) * 256], banks[b][:, 0:256], AF.Copy),
                  reads=[BK(b)], writes=[("gv", tb, t)])
                relbank(b)
        for t in range(2):
            wt, wkey = wload(wsrc(wl, 0, 16, C_GR + t * 256, 256), 16, 256)
            for j in range(2):
                c = t * 2 + j
                b = fm_group(wt, wkey, j * 128, 128)
                A("act", act(srT[:, c, :], banks[b][:, :], AF.Silu), reads=[BK(b)], writes=[("srT", c)])
                relbank(b)
        wt, wkey = wload(wsrc(wl, 0, 16, C_GA, 16), 16, 16)
        b = fm_group(wt, wkey, 0, 16)
        A("act", act(alr[0:16, :], banks[b][0:16, :], AF.Copy), reads=[BK(b)], writes=["alr"])
        relbank(b)

        ai = 0
        for tb in range(4):
            tsl = slice(tb * 128, (tb + 1) * 128)
            bg = getbank()
            A("pe", mm(banks[bg][:, 0:256], alr[0:16, tsl], aw[0:16, :], True, False), reads=["alr", "aw"], writes=[BK(bg)])
            A("pe", mm(banks[bg][:, 0:256], onesf[0:1, 0:128], ab[0:1, :], False, True), reads=["onesf", "ab"], writes=[BK(bg)])
            A("act", act(g_e1, banks[bg][:, 0:256], AF.Exp, scale=-1.0), reads=[BK(bg)], writes=["g_e1"])
            relbank(bg)
            A("act", act(g_gp, g_e1, AF.Ln, bias=1.0), reads=["g_e1"], writes=["g_gp"])
            bfm = getbank()
            for c in range(2):
                A("pe", mm(banks[bfm][:, c * 128:(c + 1) * 128], g_gp[:, c * 128:(c + 1) * 128], w_tri, True, True),
                  reads=["g_gp", "cst"], writes=[BK(bfm)])
            btm = getbank()
            A("pe", mm(banks[btm][:, 0:256], w_tri, g_gp, True, True), reads=["g_gp", "cst"], writes=[BK(btm)])
            pfm = banks[bfm][:, 0:256].rearrange("p (c t) -> p c t", c=2)
            A("act", act(g_eb, pfm, AF.Exp, scale=-1.0 / 16.0), reads=[BK(bfm)], writes=["g_eb"])
            A("act", act(g_enb, pfm, AF.Exp, scale=1.0 / 16.0), reads=[BK(bfm)], writes=["g_enb"])
            relbank(bfm)
            A("act", act(g_enbtm, banks[btm][:, 0:256], AF.Exp, scale=1.0 / 16.0), reads=[BK(btm)], writes=["g_enbtm"])
            relbank(btm)
            s2 = tb % 2
            qe = g_qe[s2]
            ke = g_ke[s2]
            ketm = g_ketm[s2]
            A("dve", stt(qe, gqT[:, :, tsl], 0.125, g_eb, ALU.mult, ALU.mult),
              reads=[("gqT", 0), ("gqT", 1), "g_eb"], writes=[("g_qe", s2)])
            A("dve", tt(ke, gkT[:, :, tsl], g_enb, ALU.mult), reads=[("gkT", 0), ("gkT", 1), "g_enb"], writes=[("g_ke", s2)])
            A("dve", tt(ketm, gktm[:, tb, :], g_enbtm, ALU.mult), reads=[("gktm", tb), "g_enbtm"], writes=[("g_ketm", s2)])
            po_b = {}
            for h in range(4):
                c = h // 2
                p0 = (h % 2) * 64
                ba = getbank()
                A("pe", mm(banks[ba][:, 0:128], ke[p0:p0 + 64, c, :], qe[p0:p0 + 64, c, :], True, True),
                  reads=[("g_ke", s2), ("g_qe", s2)], writes=[BK(ba)])
                a3 = h
                A("dve", tt(g_att[a3], banks[ba][:, 0:128], w_tri, ALU.mult), reads=[BK(ba), "cst"], writes=[("g_att", a3)])
                relbank(ba)
                po_b[h] = (getbank(), a3)
            for ch in range(2):
                csl = slice(ch * 64, (ch + 1) * 64)
                for h in range(4):
                    c = h // 2
                    p0 = (h % 2) * 64
                    bo, a3 = po_b[h]
                    A("pe", lambda e, o=banks[bo][:, ch * 64:(ch + 1) * 64],
                      l_=gstate_b[p0:p0 + 64, c, (h % 2) * 128:(h % 2 + 1) * 128], r_=qe[p0:p0 + 64, c, csl], st_=(ch == 0):
                      e.matmul(o, l_, r_, start=st_, stop=False, skip_group_check=True),
                      reads=[("gstate_b", c), ("g_qe", s2)], writes=[BK(bo)])
                for c in range(2):
                    bs_ = getbank()
                    A("pe", mm(banks[bs_][:, 0:256], ketm[csl, c * 128:(c + 1) * 128], gv[csl, tb, c * 256:(c + 1) * 256],
                               True, True),
                      reads=[("g_ketm", s2), ("gv", tb, c)], writes=[BK(bs_)])
                    A("dve", tt(gstate[:, c, :], gstate[:, c, :], banks[bs_][:, 0:256], ALU.add),
                      reads=[("gstate", c), BK(bs_)], writes=[("gstate", c)])
                    relbank(bs_)
                    A("dve", ts(gstate[:, c, :], gstate[:, c, :], g_eb[:, c, ch * 64 + 63:ch * 64 + 64], None, ALU.mult),
                      reads=[("gstate", c), "g_eb"], writes=[("gstate", c)])
                    A("dve", tcopy(gstate_b[:, c, :], gstate[:, c, :]), reads=[("gstate", c)], writes=[("gstate_b", c)])
            for h in range(4):
                bo, a3 = po_b[h]
                A("pe", mm(banks[bo][:, 0:128], gv[:, tb, h * 128:(h + 1) * 128], g_att[a3], False, True),
                  reads=[("gv", tb, h // 2), ("g_att", a3)], writes=[BK(bo)])
                A("act", act(oT[:, h, tsl], banks[bo][:, 0:128], AF.Copy), reads=[BK(bo)], writes=[("oT", h)])
                relbank(bo)
        for h in range(4):
            s2 = h % 2
            A("act", act(g_sq[s2], oT[:, h, :], AF.Square), reads=[("oT", h)], writes=[("g_sq", s2)])
            bn = getbank()
            A("pe", mm(banks[bn][:, :], onesb[:], g_sq[s2], True, True), reads=["onesb", ("g_sq", s2)], writes=[BK(bn)])
            A("act", act(g_rn[s2], banks[bn][:, :], AF.Ln, bias=128.0 * EPS), reads=[BK(bn)], writes=[("g_rn", s2)])
            relbank(bn)
            A("act", act(g_rn[s2], g_rn[s2], AF.Exp, scale=-0.5), reads=[("g_rn", s2)], writes=[("g_rn", s2)])
            A("dve", stt(oT[:, h, :], oT[:, h, :], pp[:, PP_GLN:PP_GLN + 1], g_rn[s2], ALU.mult, ALU.mult),
              reads=[("oT", h), "pp", ("g_rn", s2)], writes=[("oT", h)])
            A("dve", stt(mixedT[:, 8 + h, :], oT[:, h, :], SQRT128, srT[:, h, :], ALU.mult, ALU.mult),
              reads=[("oT", h), ("srT", h)], writes=[("mixedT", 8 + h)])
        Sd.fence()

        if "mixedT" in dbg_out and ti == dbg_tile and l == 0:
            for c in range(16):
                s = 0
                A("dve", tcopy(dbgbuf[s][:], mixedT[:, c, :]), reads=[("mixedT", c)], writes=[("dbgbuf", s)])
                dump("mixedT", dbgbuf[s][:], [("dbgbuf", s)], rows=(slice(c * 128, (c + 1) * 128), slice(None)))
            Sd.fence()

        out_proj(ti, w_out[l], 16, mixedT, lambda kc: [("mixedT", kc)], src, srcname, dst, dstname, pre, post)
        Sd.fence()

    def sweep_b(l, ti, src, srcname, dst, dstname, pre, post):
        si = 0
        for grp in range(NFF // 4):
            bsets = []
            for wmat in (w_gate[l], w_up[l]):
                bset = [getbank() for _ in range(4)]
                bsets.append(bset)
                for kh in range(2):
                    wt, wk = wload(wsrc(wmat, kh * 1024, 8, grp * 512, 512), 8, 512)
                    for c in range(4):
                        for k8 in range(8):
                            kc = kh * 8 + k8
                            A("pe", mm(banks[bset[c]][:, :], wt[:, k8, c * 128:(c + 1) * 128], hT[:, kc, :],
                                       kc == 0, kc == 15),
                              reads=[wk] + HT_ALL, writes=[BK(bset[c])])
            slots = []
            for c in range(4):
                s4 = si % 4
                si += 1
                slots.append(s4)
                A("act", act(sgb[s4], banks[bsets[0][c]][:, :], AF.Silu), reads=[BK(bsets[0][c])], writes=[("sgb", s4)])
                relbank(bsets[0][c])
            for c in range(4):
                f = grp * 4 + c
                s4 = slots[c]
                A("dve", tt(actT[:, f, :], sgb[s4], banks[bsets[1][c]][:, :], ALU.mult),
                  reads=[("sgb", s4), BK(bsets[1][c])], writes=[("actT", f)])
                relbank(bsets[1][c])
        out_proj(ti, w_down[l], NFF, actT, lambda kc: [("actT", kc)], src, srcname, dst, dstname, pre, post)
        Sd.fence()

    stages = []
    for l in range(n_layers):
        for ti in range(NTILE):
            stages.append(("A", l, ti))
        for ti in range(NTILE):
            stages.append(("B", l, ti))

    def stage_io(st):
        kind, l, ti = st
        last = l == n_layers - 1
        if kind == "A":
            src, srcn = (x_in, "x") if l == 0 else (xs, "xs")
            return src, srcn, xs, "xs"
        dst, dstn = (y_out, "y") if last else (xs, "xs")
        return xs, "xs", dst, dstn

    def make_pre(st):
        kind, l, ti = st
        gi = 0 if kind == "A" else 1
        src, srcn, _, _ = stage_io(st)

        def pre(tb):
            if tb == 0 and ti == 0:
                gsrc = g1b_d[l] if kind == "A" else g2b_d[l]
                A("sp", dma(gbuf[gi][:], gsrc), writes=[("gb", gi)], dma=f"p{gi}")
            norm_pre(src, srcn, ti, tb, gbuf[gi], ("gb", gi))
        return pre

    pre0 = make_pre(stages[0])
    for tb in range(4):
        pre0(tb)
        norm_post(tb)
    for k, st in enumerate(stages):
        kind, l, ti = st
        nxt = stages[k + 1] if k + 1 < len(stages) else None
        pre = make_pre(nxt) if nxt is not None else None
        post = norm_post if nxt is not None else None
        src, srcn, dst, dstn = stage_io(st)
        if kind == "A":
            if ti == 0:
                A("sp", dma(pp[:], pp_d[l]), writes=["pp"], dma="p2")
                A("sp", dma(aw[:], aw_d[l]), writes=["aw"], dma="p3")
                A("sp", dma(ab[:], ab_d[l]), writes=["ab"], dma="p4")
                A("pool", dma(pwb[:], pw_d[l].rearrange("g c d -> c g d")), writes=["pwb"], dma="p5")
            sweep_a(l, ti, src, srcn, dst, dstn, pre, post)
        else:
            sweep_b(l, ti, src, srcn, dst, dstn, pre, post)

    fin = A("sp", None, reads=[("y", r, n) for r in range(S // 128) for n in range(4)]
            + [k for k in Sd.lastw if isinstance(k, tuple) and k[0] == "dbg"])

    counters = Sd.finalize()

    sems = {}
    for k in counters:
        nm = "s_" + "_".join(str(v) for v in k)
        sems[k] = es.enter_context(nc.semaphore(nm))
    block = es.enter_context(nc.Block())

    def emit(engname):
        def body(eng):
            for op in Sd.ops[engname]:
                for d in op.waits:
                    eng.wait_ge(sems[d.semkey], d.count)
                if op.fn is None:
                    continue
                ins = op.fn(eng)
                if op.needs_inc:
                    ins.then_inc(sems[op.semkey], 16 if op.dma is not None else 1)
        return body

    block.tensor(emit("pe"))
    block.scalar(emit("act"))
    block.vector(emit("dve"))
    block.gpsimd(emit("pool"))
    block.sync(emit("sp"))
    es.close()
    stats = {e: len(Sd.ops[e]) for e in ENGS}
    return nc, stats


def make_consts():
    c = np.zeros((128, NCST), np.float32)
    i = np.arange(128)
    c[:, K_IDENT:K_IDENT + 128] = np.eye(128, dtype=np.float32)
    c[:, K_UINC:K_UINC + 128] = (i[:, None] >= i[None, :]).astype(np.float32)
    same = (i[:, None] // 64) == (i[None, :] // 64)
    c[:, K_TRI:K_TRI + 128] = ((i[:, None] <= i[None, :]) & same).astype(np.float32)
    c[:, K_SBM:K_SBM + 128] = (i[:, None] < i[None, :]).astype(np.float32)
    tt_ = np.arange(16)
    for g in range(4):
        w = 2 << g
        c[:, K_PRC + g * 16:K_PRC + (g + 1) * 16] = (1.0 / np.minimum(tt_ + 1, w)).astype(np.float32)[None, :]
    return c


def host_layout(inputs):
    f = lambda a: np.ascontiguousarray(np.asarray(a, dtype=np.float32))
    conv_w = f(inputs["conv_w"])
    pp = np.zeros((L, 128, NPP), np.float32)
    cw = conv_w.reshape(L, 3, 4, 128)
    pp[:, :, PP_CONV:PP_CONV + 12] = cw.transpose(0, 3, 2, 1).reshape(L, 128, 12)
    pp[:, :, PP_SBQ] = f(inputs["sb_q_g"])
    pp[:, :, PP_SBK] = f(inputs["sb_k_g"])
    pp[:, :, PP_GLN] = f(inputs["gla_norm_g"])
    pp[:, :, PP_PSC:PP_PSC + 4] = f(inputs["pool_scale"]).reshape(L, 4, 128).transpose(0, 2, 1)
    shared = {
        "w_in": f(inputs["w_in"]),
        "w_out": f(inputs["w_out"]),
        "w_gate": f(inputs["w_gate"]),
        "w_up": f(inputs["w_up"]),
        "w_down": f(inputs["w_down"]),
        "g1b": np.ascontiguousarray(np.broadcast_to(f(inputs["norm1_g"])[:, None, :], (L, 128, D))),
        "g2b": np.ascontiguousarray(np.broadcast_to(f(inputs["norm2_g"])[:, None, :], (L, 128, D))),
        "pp": pp,
        "aw": f(inputs["gla_a_w"]),
        "ab": f(inputs["gla_a_b"]).reshape(L, 1, 256),
        "pool_w": f(inputs["pool_w"]),
        "cst": make_consts(),
    }
    return shared


_CACHE = {}


def kernel(**inputs):
    x = np.ascontiguousarray(np.asarray(inputs["x"], dtype=np.float32))
    shared = host_layout(inputs)
    if "nc" not in _CACHE:
        _CACHE["nc"] = build_program(L)[0]
    nc = _CACHE["nc"]
    in_maps = [dict(shared, x=x[b]) for b in range(NCORES)]
    res = run_bass_kernel_spmd(nc, in_maps, core_ids=list(range(NCORES)))
    out = np.stack([np.asarray(r["y"], dtype=np.float32) for r in res.results], axis=0)
    return out
```

```python
import contextlib
from collections import deque

import numpy as np
import concourse.bass as bass
import concourse.mybir as mybir
from concourse.bass_utils import run_bass_kernel_spmd

F32 = mybir.dt.float32
BF16 = mybir.dt.bfloat16
AF = mybir.ActivationFunctionType
ALU = mybir.AluOpType

D = 2048
S = 2048
L = 4
NCORES = 8
INC = 5136
DFF = 5632
NFF = DFF // 128
TT = 512
NTILE = S // TT
EPS = 1e-6
NW = 4
SQRT128 = float(np.sqrt(128.0))
import os
SAME_ENGINE_SYNC = os.environ.get("K_SES", "1") == "1"

C_CONV = 0
C_SQ = 1536
C_SK = 2048
C_SV = 2560
C_GQ = 3072
C_GK = 3328
C_GV = 3584
C_GR = 4096
C_GA = 4608
C_PU = 4624

PP_CONV = 0
PP_SBQ = 12
PP_SBK = 13
PP_GLN = 14
PP_PSC = 15
NPP = 19

K_IDENT = 0
K_UINC = 128
K_TRI = 256
K_SBM = 384
K_PRC = 384 + 128
NCST = K_PRC + 64


ENGS = ("pe", "act", "dve", "pool", "sp")


class Op:
    __slots__ = ("eng", "fn", "deps", "dma", "needs_inc", "count", "semkey", "idx", "waits")


class Sched:
    def __init__(self):
        self.ops = {e: [] for e in ENGS}
        self.lastw = {}
        self.readers = {}
        self.last_by_key = {}

    def add(self, eng, fn, reads=(), writes=(), dma=None):
        op = Op()
        op.eng = eng
        op.fn = fn
        op.dma = dma
        op.semkey = ("dma", dma) if dma is not None else ("eng", eng)
        op.needs_inc = dma is not None
        op.count = None
        op.waits = None
        deps = set()
        for r in reads:
            w = self.lastw.get(r)
            if w is not None:
                deps.add(w)
        for wkey in writes:
            w = self.lastw.get(wkey)
            if w is not None:
                deps.add(w)
            rd = self.readers.get(wkey)
            if rd:
                deps.update(rd.values())
        op.deps = deps
        for r in reads:
            self.readers.setdefault(r, {})[op.semkey] = op
        for wkey in writes:
            self.lastw[wkey] = op
            self.readers[wkey] = {}
        op.idx = len(self.ops[eng])
        self.ops[eng].append(op)
        if fn is not None and dma is None:
            self.last_by_key[op.semkey] = op
        return op

    def fence(self):
        lasts = [self.last_by_key[("eng", e)] for e in ("pe", "act", "dve") if ("eng", e) in self.last_by_key]
        for e in ("pe", "act", "dve"):
            op = self.add(e, None)
            op.deps = set(lasts)

    def finalize(self):
        for e in ENGS:
            known = {}
            for op in self.ops[e]:
                need = {}
                for d in op.deps:
                    if d.dma is None and d.eng == e:
                        if e == "pe" or not SAME_ENGINE_SYNC:
                            continue
                    k = d.semkey
                    if known.get(k, -1) >= d.idx:
                        continue
                    if k not in need or need[k].idx < d.idx:
                        need[k] = d
                for k, d in need.items():
                    known[k] = d.idx
                    d.needs_inc = True
                op.waits = list(need.values())
        counters = {}
        for e in ENGS:
            for op in self.ops[e]:
                if op.needs_inc and op.fn is not None:
                    k = op.semkey
                    counters[k] = counters.get(k, 0) + (16 if op.dma is not None else 1)
                    op.count = counters[k]
        return counters


def build_program(n_layers=L, dbg=None, dbg_tile=0):
    nc = bass.Bass("TRN2", target_bir_lowering=False)
    dbg = dbg or {}

    def din(name, shape):
        return nc.dram_tensor(name, list(shape), F32, kind="ExternalInput").ap()

    x_in = din("x", (S, D))
    w_in = din("w_in", (L, D, INC))
    w_out = din("w_out", (L, D, D))
    w_gate = din("w_gate", (L, D, DFF))
    w_up = din("w_up", (L, D, DFF))
    w_down = din("w_down", (L, DFF, D))
    g1b_d = din("g1b", (L, 128, D))
    g2b_d = din("g2b", (L, 128, D))
    pp_d = din("pp", (L, 128, NPP))
    aw_d = din("aw", (L, 16, 256))
    ab_d = din("ab", (L, 1, 256))
    pw_d = din("pool_w", (L, 4, 128, 128))
    cst_d = din("cst", (128, NCST))
    y_out = nc.dram_tensor("y", [S, D], F32, kind="ExternalOutput").ap()
    xs = nc.dram_tensor("xs", [S, D], F32, kind="Internal").ap()
    dbg_out = {}
    for name, shape in dbg.items():
        dbg_out[name] = nc.dram_tensor("dbg_" + name, list(shape), F32, kind="ExternalOutput").ap()

    Sd = Sched()
    es = contextlib.ExitStack()

    def sb(name, shape, dt):
        return es.enter_context(nc.sbuf_tensor("sb_" + name, list(shape), dt))

    cst = sb("cst", (128, NCST), F32)
    identb = sb("identb", (128, 128), BF16)
    onesb = sb("onesb", (128, 128), BF16)
    onesf = sb("onesf", (128, 128), F32)
    uincb = sb("uincb", (128, 128), BF16)
    gbuf = [sb(f"gb{i}", (128, D), F32) for i in range(2)]
    pp = sb("pp", (128, NPP), F32)
    aw = sb("aw", (16, 256), F32)
    ab = sb("ab", (1, 256), F32)
    pwb = sb("pwb", (128, 4, 128), BF16)
    hT = sb("hT", (128, 16, TT), BF16)
    wslots = [sb(f"wslot{i}", (128, 16 * 256), BF16) for i in range(NW)]
    xring = [sb(f"xring{i}", (128, D), F32) for i in range(2)]
    hbring = [sb(f"hbring{i}", (128, D), BF16) for i in range(2)]
    stat = sb("stat", (128, 64), F32)
    NXO = 4
    xo = [sb(f"xo{i}", (128, 512), F32) for i in range(NXO)]
    ccar = sb("ccar", (128, 4, 2), F32)
    pcar = sb("pcar", (128, 4, 16), F32)
    dbgbuf = [sb(f"dbgbuf{i}", (128, 512), F32) for i in range(1)] if dbg else None

    UN = 51840
    U = sb("U", (128, UN), BF16)
    ucur = [0]

    def carve(nelem, dt):
        n16 = nelem * (2 if dt == F32 else 1)
        a = ucur[0]
        assert a + n16 <= UN, f"union overflow {a + n16} > {UN}"
        ucur[0] = a + n16
        v = U[:, a:a + n16]
        if dt == F32:
            v = v.bitcast(F32)
        return v

    banks = [es.enter_context(nc.psum_tensor(f"bank{i}", [128, 512], F32)) for i in range(8)]
    freeb = deque(range(8))

    def getbank():
        return freeb.popleft()

    def relbank(b):
        freeb.append(b)

    def BK(b):
        return ("B", b)

    def A(eng, fn, reads=(), writes=(), dma=None):
        return Sd.add(eng, fn, reads, writes, dma)

    def mm(out, lhsT, rhs, start, stop):
        return lambda e: e.matmul(out, lhsT, rhs, start=start, stop=stop)

    def act(out, in_, func, **kw):
        return lambda e: e.activation(out=out, in_=in_, func=func, **kw)

    def tcopy(out, in_):
        return lambda e: e.tensor_copy(out=out, in_=in_)

    def tt(out, in0, in1, op):
        return lambda e: e.tensor_tensor(out=out, in0=in0, in1=in1, op=op)

    def ts(out, in0, s1, s2, op0, op1=None):
        if op1 is None:
            return lambda e: e.tensor_scalar(out=out, in0=in0, scalar1=s1, scalar2=None, op0=op0)
        return lambda e: e.tensor_scalar(out=out, in0=in0, scalar1=s1, scalar2=s2, op0=op0, op1=op1)

    def stt(out, in0, scalar, in1, op0, op1):
        return lambda e: e.scalar_tensor_tensor(out=out, in0=in0, scalar=scalar, in1=in1, op0=op0, op1=op1)

    def dma(out, in_):
        return lambda e: e.dma_start(out=out, in_=in_)

    def memset(ap, v):
        return lambda e: e.memset(ap, v)

    dbg_n = [0]

    def dump(name, src_ap, reads, rows=None):
        if name not in dbg_out:
            return
        dst = dbg_out[name]
        dbg_n[0] += 1
        A("sp", dma(dst if rows is None else dst[rows], src_ap), reads=reads, writes=[("dbg", name, dbg_n[0])],
          dma=f"dbg{dbg_n[0] % 4}")

    stat_i = [0]

    def statcol():
        c = stat_i[0] % 64
        stat_i[0] += 1
        return c

    A("sp", dma(cst[:], cst_d[:, :]), writes=["cst"], dma="c0")
    A("dve", tcopy(identb[:], cst[:, K_IDENT:K_IDENT + 128]), reads=["cst"], writes=["identb"])
    A("dve", tcopy(uincb[:], cst[:, K_UINC:K_UINC + 128]), reads=["cst"], writes=["uincb"])
    A("dve", memset(onesb[:], 1.0), writes=["onesb"])
    A("dve", memset(onesf[:], 1.0), writes=["onesf"])
    A("dve", memset(stat[:], 12345.0), writes=[("stat", c) for c in range(64)])

    wn = [0]

    def wload(src3, nk, ncol):
        s = wn[0] % NW
        wn[0] += 1
        view = wslots[s][:, 0:nk * ncol].rearrange("p (k n) -> p k n", k=nk)
        A("pool", dma(view, src3), writes=[("w", s)], dma=f"w{s}")
        return view, ("w", s)

    def wsrc(wl, r0, nk, c0, ncol):
        return wl[r0:r0 + nk * 128, c0:c0 + ncol].rearrange("(k p) n -> p k n", p=128)

    HT_ALL = [("hT", tb) for tb in range(4)]

    def fm_group(wt, wkey, off, M, nk=16, rhs_of=None, rhs_keys=None):
        b = getbank()
        for kc in range(nk):
            rhs = hT[:, kc, :] if rhs_of is None else rhs_of(kc)
            A("pe", mm(banks[b][0:M, :], wt[:, kc, off:off + M], rhs, kc == 0, kc == nk - 1),
              reads=[wkey] + (HT_ALL if rhs_keys is None else rhs_keys), writes=[BK(b)])
        return b

    def tm_group(wt, wkey, tb, ncol):
        b = getbank()
        for kc in range(16):
            A("pe", mm(banks[b][:, 0:ncol], hT[:, kc, tb * 128:(tb + 1) * 128], wt[:, kc, 0:ncol], kc == 0, kc == 15),
              reads=[wkey, ("hT", tb)], writes=[BK(b)])
        return b

    xn = [0]
    evac_flip = [0]

    norm_slot = {}

    def norm_pre(src, srcname, ti, tb, gb, gbkey):
        r = ti * 4 + tb
        sl = xn[0] % 2
        xn[0] += 1
        norm_slot[tb] = sl
        xk = ("xring", sl)
        hk = ("hbring", sl)
        A("sp", dma(xring[sl][:], src[r * 128:(r + 1) * 128, :]),
          reads=[(srcname, r, n) for n in range(4)], writes=[xk], dma=f"xl{sl}")
        c = statcol()
        A("act", act(hbring[sl][:], xring[sl][:], AF.Square, accum_out=stat[:, c:c + 1]),
          reads=[xk], writes=[hk, ("stat", c)])
        A("act", act(stat[:, c:c + 1], stat[:, c:c + 1], AF.Ln, scale=1.0 / D, bias=EPS),
          reads=[("stat", c)], writes=[("stat", c)])
        A("act", act(stat[:, c:c + 1], stat[:, c:c + 1], AF.Exp, scale=-0.5),
          reads=[("stat", c)], writes=[("stat", c)])
        A("dve", stt(hbring[sl][:], xring[sl][:], stat[:, c:c + 1], gb[:], ALU.mult, ALU.mult),
          reads=[xk, ("stat", c), gbkey], writes=[hk])

    def norm_post(tb):
        sl = norm_slot[tb]
        hk = ("hbring", sl)
        for half in range(2):
            b = getbank()
            bv = banks[b][:].bitcast(BF16)
            for k8 in range(8):
                kc = half * 8 + k8
                A("pe", lambda e, o=bv[:, k8 * 128:(k8 + 1) * 128], i=hbring[sl][:, kc * 128:(kc + 1) * 128]:
                  e.transpose(out=o, in_=i, identity=identb[:]),
                  reads=[hk, "identb"], writes=[BK(b)])
            src_v = bv[:, 0:1024].rearrange("p (k t) -> p k t", k=8)
            dst_v = hT[:, half * 8:(half + 1) * 8, tb * 128:(tb + 1) * 128]
            eng = "act" if (evac_flip[0] % 2 == 0) else "dve"
            evac_flip[0] += 1
            if eng == "act":
                A("act", act(dst_v, src_v, AF.Copy), reads=[BK(b)], writes=[("hT", tb)])
            else:
                A("dve", tcopy(dst_v, src_v), reads=[BK(b)], writes=[("hT", tb)])
            relbank(b)

    xon = [0]

    def out_proj(ti, wl, nkc, actT, actkeys_of, src, srcname, dst, dstname, pre=None, post=None):
        ktiles = [(k0, min(8, nkc - k0)) for k0 in range(0, nkc, 8)]
        for ng in range(4):
            xslots = []
            for tb in range(4):
                r = ti * 4 + tb
                s = xon[0] % NXO
                xon[0] += 1
                xslots.append(s)
                A("act", dma(xo[s][:], src[r * 128:(r + 1) * 128, ng * 512:(ng + 1) * 512]),
                  reads=[(srcname, r, ng)], writes=[("xo", s)], dma=f"xo{s}")
            if pre is not None:
                pre(ng)
            bs = [getbank() for _ in range(4)]
            for (k0, nk) in ktiles:
                wt, wkey = wload(wsrc(wl, k0 * 128, nk, ng * 512, 512), nk, 512)
                for tb in range(4):
                    for k8 in range(nk):
                        kc = k0 + k8
                        A("pe", mm(banks[bs[tb]][:, :], actT[:, kc, tb * 128:(tb + 1) * 128], wt[:, k8, :],
                                   kc == 0, kc == nkc - 1),
                          reads=[wkey] + actkeys_of(kc), writes=[BK(bs[tb])])
            if post is not None and ng > 0:
                post(ng - 1)
            for tb in range(4):
                r = ti * 4 + tb
                s = xslots[tb]
                A("dve", tt(xo[s][:], xo[s][:], banks[bs[tb]][:, :], ALU.add),
                  reads=[("xo", s), BK(bs[tb])], writes=[("xo", s)])
                A("sp", dma(dst[r * 128:(r + 1) * 128, ng * 512:(ng + 1) * 512], xo[s][:]),
                  reads=[("xo", s)], writes=[(dstname, r, ng)], dma=f"xs{s}")
                relbank(bs[tb])
        if post is not None:
            post(3)

    ucur[0] = 0
    mixedT = carve(16 * TT, BF16).rearrange("p (c t) -> p c t", c=16)
    kTc = carve(4 * S, BF16).rearrange("p (h t) -> p h t", h=4)
    Vc = carve(16 * 512, BF16).rearrange("p (b c) -> p b c", b=16)
    gstate = carve(2 * 256, F32).rearrange("p (c v) -> p c v", c=2)
    gstate_b = carve(2 * 256, BF16).rearrange("p (c v) -> p c v", c=2)
    a_mix_base = ucur[0]
    ubuf = carve(4 * 514, F32).rearrange("p (c t) -> p c t", c=4)
    ybuf = carve(4 * 512, F32).rearrange("p (c t) -> p c t", c=4)
    cbuf = carve(4 * 512, F32).rearrange("p (c t) -> p c t", c=4)
    conv_end = ucur[0]
    ubp = carve(4 * 528, F32).rearrange("p (c t) -> p c t", c=4)
    plev = [carve(528, F32) for _ in range(4)]
    pooled_f = [carve(512, F32) for _ in range(2)]
    pooled_b = [carve(512, BF16) for _ in range(2)]
    pool_end = ucur[0]
    ucur[0] = a_mix_base
    qT = carve(4 * 512, BF16).rearrange("p (h t) -> p h t", h=4)
    qf = [carve(512, F32) for _ in range(2)]
    sqb = [carve(512, BF16) for _ in range(2)]
    rsb = [carve(512, F32) for _ in range(2)]
    NEB = 4
    Eb = [carve(512, F32) for _ in range(NEB)]
    spb = [carve(512, BF16) for _ in range(NEB)]
    cumb = [carve(512, F32) for _ in range(2)]
    Xb = [carve(512, F32) for _ in range(2)]
    Ab = [carve(512, BF16) for _ in range(NEB)]
    totb = carve(512, F32)
    sb_end = ucur[0]
    ucur[0] = a_mix_base
    gqT = carve(2 * 512, F32).rearrange("p (c t) -> p c t", c=2)
    gkT = carve(2 * 512, F32).rearrange("p (c t) -> p c t", c=2)
    gktm = carve(4 * 256, F32).rearrange("p (b c) -> p b c", b=4)
    gv = carve(4 * 512, BF16).rearrange("p (b c) -> p b c", b=4)
    srT = carve(4 * 512, F32).rearrange("p (c t) -> p c t", c=4)
    alr = carve(512, F32)
    oT = carve(4 * 512, F32).rearrange("p (h t) -> p h t", h=4)
    g_e1 = carve(256, F32)
    g_gp = carve(256, F32)
    g_eb = carve(256, F32).rearrange("p (c t) -> p c t", c=2)
    g_enb = carve(256, F32).rearrange("p (c t) -> p c t", c=2)
    g_enbtm = carve(256, F32)
    g_qe = [carve(256, BF16).rearrange("p (c t) -> p c t", c=2) for _ in range(2)]
    g_ke = [carve(256, BF16).rearrange("p (c t) -> p c t", c=2) for _ in range(2)]
    g_ketm = [carve(256, BF16) for _ in range(2)]
    g_att = [carve(128, BF16) for _ in range(4)]
    g_tmp = carve(256, F32)
    g_sq = [carve(512, BF16) for _ in range(2)]
    g_rn = [carve(512, F32) for _ in range(2)]
    gla_end = ucur[0]
    a_end = max(conv_end, pool_end, sb_end, gla_end)
    ucur[0] = 0
    actT = carve(NFF * TT, BF16).rearrange("p (c t) -> p c t", c=NFF)
    sgb = [carve(512, F32) for _ in range(4)]
    b_end = ucur[0]
    assert max(a_end, b_end) <= UN

    w_tri = cst[:, K_TRI:K_TRI + 128]

    def sweep_a(l, ti, src, srcname, dst, dstname, pre, post):
        t0 = ti * TT
        wl = w_in[l]

        if ti == 0:
            A("dve", memset(ccar[:], 0.0), writes=["ccar"])
        A("dve", tcopy(ubuf[:, :, 0:2], ccar[:]), reads=["ccar"], writes=[("ubuf", c) for c in range(4)])
        for t in (2, 3, 4, 5, 0, 1):
            wt, wkey = wload(wsrc(wl, 0, 16, C_CONV + t * 256, 256), 16, 256)
            for j in range(2):
                c = (t % 2) * 2 + j
                b = fm_group(wt, wkey, j * 128, 128)
                if 2 <= t < 4:
                    A("act", act(cbuf[:, c, :], banks[b][:, :], AF.Copy), reads=[BK(b)], writes=[("cbuf", c)])
                elif t >= 4:
                    yt = ybuf[:, c, :]
                    yk = ("ybuf", c)
                    A("dve", tt(ubuf[:, c, 2:514], cbuf[:, c, :], banks[b][:, :], ALU.mult),
                      reads=[("cbuf", c), BK(b)], writes=[("ubuf", c)])
                    A("dve", ts(yt, ubuf[:, c, 2:514], pp[:, PP_CONV + c * 3 + 2:PP_CONV + c * 3 + 3], None, ALU.mult),
                      reads=[("ubuf", c), "pp"], writes=[yk])
                    A("dve", stt(yt, ubuf[:, c, 1:513], pp[:, PP_CONV + c * 3 + 1:PP_CONV + c * 3 + 2], yt,
                                 ALU.mult, ALU.add), reads=[("ubuf", c), "pp", yk], writes=[yk])
                    A("dve", stt(yt, ubuf[:, c, 0:512], pp[:, PP_CONV + c * 3 + 0:PP_CONV + c * 3 + 1], yt,
                                 ALU.mult, ALU.add), reads=[("ubuf", c), "pp", yk], writes=[yk])
                    A("dve", tcopy(ccar[:, c, :], ubuf[:, c, 512:514]), reads=[("ubuf", c)], writes=["ccar"])
                else:
                    A("dve", tt(mixedT[:, c, :], ybuf[:, c, :], banks[b][:, :], ALU.mult),
                      reads=[("ybuf", c), BK(b)], writes=[("mixedT", c)])
                relbank(b)

        if ti == 0:
            A("dve", memset(pcar[:], 0.0), writes=["pcar"])
        A("dve", tcopy(ubp[:, :, 0:16], pcar[:]), reads=["pcar"], writes=[("ubp", g) for g in range(4)])
        for t in range(2):
            wt, wkey = wload(wsrc(wl, 0, 16, C_PU + t * 256, 256), 16, 256)
            for j in range(2):
                g = t * 2 + j
                win = 2 << g
                b = fm_group(wt, wkey, j * 128, 128)
                A("act", act(ubp[:, g, 16:528], banks[b][:, :], AF.Copy), reads=[BK(b)], writes=[("ubp", g)])
                relbank(b)
        for t in range(2):
            for j in range(2):
                g = t * 2 + j
                win = 2 << g
                prev = ubp[:, g, :]
                prevk = ("ubp", g)
                sh = 1
                lo = 1
                for lev in range(g + 1):
                    cur = plev[lev]
                    A("dve", tt(cur[:, lo:528], prev[:, lo:528], prev[:, lo - sh:528 - sh], ALU.add),
                      reads=[prevk], writes=[("plev", lev)])
                    prev = cur
                    prevk = ("plev", lev)
                    sh *= 2
                    lo += sh
                pf = pooled_f[g % 2]
                pb_ = pooled_b[g % 2]
                A("dve", stt(pf, prev[:, 16:528], 1.0 / win, ubp[:, g, 16:528], ALU.mult, ALU.subtract),
                  reads=[prevk, ("ubp", g)], writes=[("pooled_f", g % 2)])
                if ti == 0:
                    A("dve", tt(pf[:, 0:16], prev[:, 16:32], cst[:, K_PRC + g * 16:K_PRC + (g + 1) * 16], ALU.mult),
                      reads=[prevk, "cst", ("pooled_f", g % 2)], writes=[("pooled_f", g % 2)])
                    A("dve", tt(pf[:, 0:16], pf[:, 0:16], ubp[:, g, 16:32], ALU.subtract),
                      reads=[("ubp", g), ("pooled_f", g % 2)], writes=[("pooled_f", g % 2)])
                A("dve", tcopy(pb_, pf), reads=[("pooled_f", g % 2)], writes=[("pooled_b", g % 2)])
                A("dve", tcopy(pcar[:, g, :], ubp[:, g, 512:528]), reads=[("ubp", g)], writes=["pcar"])
                b2 = getbank()
                A("pe", mm(banks[b2][:, :], pwb[:, g, :], pb_, True, True),
                  reads=["pwb", ("pooled_b", g % 2)], writes=[BK(b2)])
                A("act", act(mixedT[:, 12 + g, :], banks[b2][:, :], AF.Copy, scale=pp[:, PP_PSC + g:PP_PSC + g + 1]),
                  reads=[BK(b2), "pp"], writes=[("mixedT", 12 + g)])
                relbank(b2)
        Sd.fence()

        qi = 0
        for t in range(4):
            wt, wkey = wload(wsrc(wl, 0, 16, C_SQ + t * 256, 256), 16, 256)
            for j in range(2):
                h = (t % 2) * 2 + j
                isq = t < 2
                b = fm_group(wt, wkey, j * 128, 128)
                s2 = qi % 2
                qi += 1
                A("act", act(qf[s2], banks[b][:, :], AF.Copy), reads=[BK(b)], writes=[("qf", s2)])
                A("act", act(sqb[s2], banks[b][:, :], AF.Square), reads=[BK(b)], writes=[("sqb", s2)])
                relbank(b)
                b2 = getbank()
                A("pe", mm(banks[b2][:, :], onesb[:], sqb[s2], True, True), reads=["onesb", ("sqb", s2)], writes=[BK(b2)])
                A("act", act(rsb[s2], banks[b2][:, :], AF.Ln, bias=128.0 * EPS), reads=[BK(b2)], writes=[("rsb", s2)])
                relbank(b2)
                A("act", act(rsb[s2], rsb[s2], AF.Exp, scale=-0.5), reads=[("rsb", s2)], writes=[("rsb", s2)])
                gcol = PP_SBQ if isq else PP_SBK
                if isq:
                    dstv = qT[:, h, :]
                    dk = ("qT", h)
                else:
                    dstv = kTc[:, h, t0:t0 + TT]
                    dk = ("kT", h, ti)
                A("dve", stt(dstv, qf[s2], pp[:, gcol:gcol + 1], rsb[s2], ALU.mult, ALU.mult),
                  reads=[("qf", s2), "pp", ("rsb", s2)], writes=[dk])
        for t in range(2):
            wt, wkey = wload(wsrc(wl, 0, 16, C_SV + t * 256, 256), 16, 256)
            for tb in range(4):
                b = tm_group(wt, wkey, tb, 256)
                A("act", act(Vc[:, ti * 4 + tb, t * 256:(t + 1) * 256], banks[b][:, 0:256], AF.Copy),
                  reads=[BK(b)], writes=[("Vc", ti * 4 + tb, t)])
                relbank(b)
        pairs = [(h, kb) for h in range(4) for kb in range(ti * 4 + 3, -1, -1)]
        NP = len(pairs)
        st = {}
        po_bank = {}

        def stage_a(n):
            h, kb = pairs[n]
            j = kb - ti * 4
            q0 = max(j, 0) * 128
            e = n % NEB
            bz = getbank()
            A("pe", mm(banks[bz][:, q0:512], kTc[:, h, kb * 128:(kb + 1) * 128], qT[:, h, q0:512], True, True),
              reads=[("kT", h, kb // 4), ("qT", h)], writes=[BK(bz)])
            A("act", act(Eb[e][:, q0:512], banks[bz][:, q0:512], AF.Exp, scale=SQRT128), reads=[BK(bz)], writes=[("E", e)])
            relbank(bz)
            if j >= 0:
                A("dve", tt(Eb[e][:, q0:q0 + 128], Eb[e][:, q0:q0 + 128], cst[:, K_SBM:K_SBM + 128], ALU.mult),
                  reads=[("E", e), "cst"], writes=[("E", e)])
            A("act", act(spb[e][:, q0:512], Eb[e][:, q0:512], AF.Ln, bias=1.0), reads=[("E", e)], writes=[("sp", e)])

        def stage_b(n):
            h, kb = pairs[n]
            j = kb - ti * 4
            q0 = max(j, 0) * 128
            e = n % NEB
            first = kb == ti * 4 + 3
            last = kb == 0
            if first:
                A("dve", memset(totb, 0.0), writes=["tot"])
            bc = getbank()
            A("pe", mm(banks[bc][:, q0:512], uincb[:], spb[e][:, q0:512], True, True), reads=["uincb", ("sp", e)], writes=[BK(bc)])
            c2 = n % 2
            A("dve", tt(cumb[c2][:, q0:512], banks[bc][:, q0:512], totb[:, q0:512], ALU.add),
              reads=[BK(bc), "tot"], writes=[("cum", c2)])
            relbank(bc)
            if not last:
                bo = getbank()
                A("pe", mm(banks[bo][:, q0:512], onesb[:], spb[e][:, q0:512], True, True), reads=["onesb", ("sp", e)], writes=[BK(bo)])
                A("dve", tt(totb[:, q0:512], totb[:, q0:512], banks[bo][:, q0:512], ALU.add), reads=[BK(bo), "tot"], writes=["tot"])
                relbank(bo)
            A("act", act(Xb[c2][:, q0:512], cumb[c2][:, q0:512], AF.Exp, scale=-1.0), reads=[("cum", c2)], writes=[("X", c2)])
            if q0 > 0:
                A("pool", memset(Ab[e][:, 0:q0], 0.0), writes=[("A", e)])
            A("pool", tt(Ab[e][:, q0:512], Eb[e][:, q0:512], Xb[c2][:, q0:512], ALU.mult), reads=[("E", e), ("X", c2)], writes=[("A", e)])

        def stage_c(n):
            h, kb = pairs[n]
            e = n % NEB
            first = kb == ti * 4 + 3
            last = kb == 0
            if first:
                po_bank[h] = getbank()
            bp = po_bank[h]
            A("pe", mm(banks[bp][:, :], Vc[:, kb, h * 128:(h + 1) * 128], Ab[e], first, last),
              reads=[("Vc", kb, h // 2), ("A", e)], writes=[BK(bp)])
            if last:
                A("act", act(mixedT[:, 4 + h, :], banks[bp][:, :], AF.Copy), reads=[BK(bp)], writes=[("mixedT", 4 + h)])
                relbank(bp)

        gla_pref = [wload(wsrc(wl, 0, 16, C_GQ, 256), 16, 256),
                    wload(wsrc(wl, 0, 16, C_GK, 256), 16, 256),
                    wload(wsrc(wl, 0, 16, C_GV, 256), 16, 256)]
        for n in range(NP + 3):
            if n < NP:
                stage_a(n)
            if 0 <= n - 1 < NP:
                stage_b(n - 1)
            if 0 <= n - 3 < NP:
                stage_c(n - 3)
        Sd.fence()

        if ti == 0:
            A("dve", memset(gstate[:], 0.0), writes=[("gstate", c) for c in range(2)])
            A("dve", memset(gstate_b[:], 0.0), writes=[("gstate_b", c) for c in range(2)])
        wt, wkey = gla_pref[0]
        for c in range(2):
            b = fm_group(wt, wkey, c * 128, 128)
            A("act", act(gqT[:, c, :], banks[b][:, :], AF.Copy), reads=[BK(b)], writes=[("gqT", c)])
            relbank(b)
        wt, wkey = gla_pref[1]
        for c in range(2):
            b = fm_group(wt, wkey, c * 128, 128)
            A("act", act(gkT[:, c, :], banks[b][:, :], AF.Copy), reads=[BK(b)], writes=[("gkT", c)])
            relbank(b)
        for tb in range(4):
            b = tm_group(wt, wkey, tb, 256)
            A("act", act(gktm[:, tb, :], banks[b][:, 0:256], AF.Copy), reads=[BK(b)], writes=[("gktm", tb)])
            relbank(b)
        for t in range(2):
            wt, wkey = gla_pref[2] if t == 0 else wload(wsrc(wl, 0, 16, C_GV + t * 256, 256), 16, 256)
            for tb in range(4):
                b = tm_group(wt, wkey, tb, 256)
                A("act", act(gv[:, tb, t * 256:(t + 1) * 256], banks[b][:, 0:256], AF.Copy),
                  reads=[BK(b)], writes=[("gv", tb, t)])
                relbank(b)
        for t in range(2):
            wt, wkey = wload(wsrc(wl, 0, 16, C_GR + t * 256, 256), 16, 256)
            for j in range(2):
                c = t * 2 + j
                b = fm_group(wt, wkey, j * 128, 128)
                A("act", act(srT[:, c, :], banks[b][:, :], AF.Silu), reads=[BK(b)], writes=[("srT", c)])
                relbank(b)
        wt, wkey = wload(wsrc(wl, 0, 16, C_GA, 16), 16, 16)
        b = fm_group(wt, wkey, 0, 16)
        A("act", act(alr[0:16, :], banks[b][0:16, :], AF.Copy), reads=[BK(b)], writes=["alr"])
        relbank(b)

        ai = 0
        for tb in range(4):
            tsl = slice(tb * 128, (tb + 1) * 128)
            bg = getbank()
            A("pe", mm(banks[bg][:, 0:256], alr[0:16, tsl], aw[0:16, :], True, False), reads=["alr", "aw"], writes=[BK(bg)])
            A("pe", mm(banks[bg][:, 0:256], onesf[0:1, 0:128], ab[0:1, :], False, True), reads=["onesf", "ab"], writes=[BK(bg)])
            A("act", act(g_e1, banks[bg][:, 0:256], AF.Exp, scale=-1.0), reads=[BK(bg)], writes=["g_e1"])
            relbank(bg)
            A("act", act(g_gp, g_e1, AF.Ln, bias=1.0), reads=["g_e1"], writes=["g_gp"])
            bfm = getbank()
            for c in range(2):
                A("pe", mm(banks[bfm][:, c * 128:(c + 1) * 128], g_gp[:, c * 128:(c + 1) * 128], w_tri, True, True),
                  reads=["g_gp", "cst"], writes=[BK(bfm)])
            btm = getbank()
            A("pe", mm(banks[btm][:, 0:256], w_tri, g_gp, True, True), reads=["g_gp", "cst"], writes=[BK(btm)])
            pfm = banks[bfm][:, 0:256].rearrange("p (c t) -> p c t", c=2)
            A("act", act(g_eb, pfm, AF.Exp, scale=-1.0 / 16.0), reads=[BK(bfm)], writes=["g_eb"])
            A("act", act(g_enb, pfm, AF.Exp, scale=1.0 / 16.0), reads=[BK(bfm)], writes=["g_enb"])
            relbank(bfm)
            A("act", act(g_enbtm, banks[btm][:, 0:256], AF.Exp, scale=1.0 / 16.0), reads=[BK(btm)], writes=["g_enbtm"])
            relbank(btm)
            s2 = tb % 2
            qe = g_qe[s2]
            ke = g_ke[s2]
            ketm = g_ketm[s2]
            A("dve", stt(qe, gqT[:, :, tsl], 0.125, g_eb, ALU.mult, ALU.mult),
              reads=[("gqT", 0), ("gqT", 1), "g_eb"], writes=[("g_qe", s2)])
            A("dve", tt(ke, gkT[:, :, tsl], g_enb, ALU.mult), reads=[("gkT", 0), ("gkT", 1), "g_enb"], writes=[("g_ke", s2)])
            A("dve", tt(ketm, gktm[:, tb, :], g_enbtm, ALU.mult), reads=[("gktm", tb), "g_enbtm"], writes=[("g_ketm", s2)])
            po_b = {}
            for h in range(4):
                c = h // 2
                p0 = (h % 2) * 64
                ba = getbank()
                A("pe", mm(banks[ba][:, 0:128], ke[p0:p0 + 64, c, :], qe[p0:p0 + 64, c, :], True, True),
                  reads=[("g_ke", s2), ("g_qe", s2)], writes=[BK(ba)])
                a3 = h
                A("dve", tt(g_att[a3], banks[ba][:, 0:128], w_tri, ALU.mult), reads=[BK(ba), "cst"], writes=[("g_att", a3)])
                relbank(ba)
                po_b[h] = (getbank(), a3)
            for ch in range(2):
                csl = slice(ch * 64, (ch + 1) * 64)
                for h in range(4):
                    c = h // 2
                    p0 = (h % 2) * 64
                    bo, a3 = po_b[h]
                    A("pe", lambda e, o=banks[bo][:, ch * 64:(ch + 1) * 64],
                      l_=gstate_b[p0:p0 + 64, c, (h % 2) * 128:(h % 2 + 1) * 128], r_=qe[p0:p0 + 64, c, csl], st_=(ch == 0):
                      e.matmul(o, l_, r_, start=st_, stop=False, skip_group_check=True),
                      reads=[("gstate_b", c), ("g_qe", s2)], writes=[BK(bo)])
                for c in range(2):
                    bs_ = getbank()
                    A("pe", mm(banks[bs_][:, 0:256], ketm[csl, c * 128:(c + 1) * 128], gv[csl, tb, c * 256:(c + 1) * 256],
                               True, True),
                      reads=[("g_ketm", s2), ("gv", tb, c)], writes=[BK(bs_)])
                    A("dve", tt(gstate[:, c, :], gstate[:, c, :], banks[bs_][:, 0:256], ALU.add),
                      reads=[("gstate", c), BK(bs_)], writes=[("gstate", c)])
                    relbank(bs_)
                    A("dve", ts(gstate[:, c, :], gstate[:, c, :], g_eb[:, c, ch * 64 + 63:ch * 64 + 64], None, ALU.mult),
                      reads=[("gstate", c), "g_eb"], writes=[("gstate", c)])
                    A("dve", tcopy(gstate_b[:, c, :], gstate[:, c, :]), reads=[("gstate", c)], writes=[("gstate_b", c)])
            for h in range(4):
                bo, a3 = po_b[h]
                A("pe", mm(banks[bo][:, 0:128], gv[:, tb, h * 128:(h + 1) * 128], g_att[a3], False, True),
                  reads=[("gv", tb, h // 2), ("g_att", a3)], writes=[BK(bo)])
                A("act", act(oT[:, h, tsl], banks[bo][:, 0:128], AF.Copy), reads=[BK(bo)], writes=[("oT", h)])
                relbank(bo)
        for h in range(4):
            s2 = h % 2
            A("act", act(g_sq[s2], oT[:, h, :], AF.Square), reads=[("oT", h)], writes=[("g_sq", s2)])
            bn = getbank()
            A("pe", mm(banks[bn][:, :], onesb[:], g_sq[s2], True, True), reads=["onesb", ("g_sq", s2)], writes=[BK(bn)])
            A("act", act(g_rn[s2], banks[bn][:, :], AF.Ln, bias=128.0 * EPS), reads=[BK(bn)], writes=[("g_rn", s2)])
            relbank(bn)
            A("act", act(g_rn[s2], g_rn[s2], AF.Exp, scale=-0.5), reads=[("g_rn", s2)], writes=[("g_rn", s2)])
            A("dve", stt(oT[:, h, :], oT[:, h, :], pp[:, PP_GLN:PP_GLN + 1], g_rn[s2], ALU.mult, ALU.mult),
              reads=[("oT", h), "pp", ("g_rn", s2)], writes=[("oT", h)])
            A("dve", stt(mixedT[:, 8 + h, :], oT[:, h, :], SQRT128, srT[:, h, :], ALU.mult, ALU.mult),
              reads=[("oT", h), ("srT", h)], writes=[("mixedT", 8 + h)])
        Sd.fence()

        if "mixedT" in dbg_out and ti == dbg_tile and l == 0:
            for c in range(16):
                s = 0
                A("dve", tcopy(dbgbuf[s][:], mixedT[:, c, :]), reads=[("mixedT", c)], writes=[("dbgbuf", s)])
                dump("mixedT", dbgbuf[s][:], [("dbgbuf", s)], rows=(slice(c * 128, (c + 1) * 128), slice(None)))
            Sd.fence()

        out_proj(ti, w_out[l], 16, mixedT, lambda kc: [("mixedT", kc)], src, srcname, dst, dstname, pre, post)
        Sd.fence()

    def sweep_b(l, ti, src, srcname, dst, dstname, pre, post):
        si = 0
        for grp in range(NFF // 4):
            bsets = []
            for wmat in (w_gate[l], w_up[l]):
                bset = [getbank() for _ in range(4)]
                bsets.append(bset)
                for kh in range(2):
                    wt, wk = wload(wsrc(wmat, kh * 1024, 8, grp * 512, 512), 8, 512)
                    for c in range(4):
                        for k8 in range(8):
                            kc = kh * 8 + k8
                            A("pe", mm(banks[bset[c]][:, :], wt[:, k8, c * 128:(c + 1) * 128], hT[:, kc, :],
                                       kc == 0, kc == 15),
                              reads=[wk] + HT_ALL, writes=[BK(bset[c])])
            slots = []
            for c in range(4):
                s4 = si % 4
                si += 1
                slots.append(s4)
                A("act", act(sgb[s4], banks[bsets[0][c]][:, :], AF.Silu), reads=[BK(bsets[0][c])], writes=[("sgb", s4)])
                relbank(bsets[0][c])
            for c in range(4):
                f = grp * 4 + c
                s4 = slots[c]
                A("dve", tt(actT[:, f, :], sgb[s4], banks[bsets[1][c]][:, :], ALU.mult),
                  reads=[("sgb", s4), BK(bsets[1][c])], writes=[("actT", f)])
                relbank(bsets[1][c])
        out_proj(ti, w_down[l], NFF, actT, lambda kc: [("actT", kc)], src, srcname, dst, dstname, pre, post)
        Sd.fence()

    stages = []
    for l in range(n_layers):
        for ti in range(NTILE):
            stages.append(("A", l, ti))
        for ti in range(NTILE):
            stages.append(("B", l, ti))

    def stage_io(st):
        kind, l, ti = st
        last = l == n_layers - 1
        if kind == "A":
            src, srcn = (x_in, "x") if l == 0 else (xs, "xs")
            return src, srcn, xs, "xs"
        dst, dstn = (y_out, "y") if last else (xs, "xs")
        return xs, "xs", dst, dstn

    def make_pre(st):
        kind, l, ti = st
        gi = 0 if kind == "A" else 1
        src, srcn, _, _ = stage_io(st)

        def pre(tb):
            if tb == 0 and ti == 0:
                gsrc = g1b_d[l] if kind == "A" else g2b_d[l]
                A("sp", dma(gbuf[gi][:], gsrc), writes=[("gb", gi)], dma=f"p{gi}")
            norm_pre(src, srcn, ti, tb, gbuf[gi], ("gb", gi))
        return pre

    pre0 = make_pre(stages[0])
    for tb in range(4):
        pre0(tb)
        norm_post(tb)
    for k, st in enumerate(stages):
        kind, l, ti = st
        nxt = stages[k + 1] if k + 1 < len(stages) else None
        pre = make_pre(nxt) if nxt is not None else None
        post = norm_post if nxt is not None else None
        src, srcn, dst, dstn = stage_io(st)
        if kind == "A":
            if ti == 0:
                A("sp", dma(pp[:], pp_d[l]), writes=["pp"], dma="p2")
                A("sp", dma(aw[:], aw_d[l]), writes=["aw"], dma="p3")
                A("sp", dma(ab[:], ab_d[l]), writes=["ab"], dma="p4")
                A("pool", dma(pwb[:], pw_d[l].rearrange("g c d -> c g d")), writes=["pwb"], dma="p5")
            sweep_a(l, ti, src, srcn, dst, dstn, pre, post)
        else:
            sweep_b(l, ti, src, srcn, dst, dstn, pre, post)

    fin = A("sp", None, reads=[("y", r, n) for r in range(S // 128) for n in range(4)]
            + [k for k in Sd.lastw if isinstance(k, tuple) and k[0] == "dbg"])

    counters = Sd.finalize()

    sems = {}
    for k in counters:
        nm = "s_" + "_".join(str(v) for v in k)
        sems[k] = es.enter_context(nc.semaphore(nm))
    block = es.enter_context(nc.Block())

    def emit(engname):
        def body(eng):
            for op in Sd.ops[engname]:
                for d in op.waits:
                    eng.wait_ge(sems[d.semkey], d.count)
                if op.fn is None:
                    continue
                ins = op.fn(eng)
                if op.needs_inc:
                    ins.then_inc(sems[op.semkey], 16 if op.dma is not None else 1)
        return body

    block.tensor(emit("pe"))
    block.scalar(emit("act"))
    block.vector(emit("dve"))
    block.gpsimd(emit("pool"))
    block.sync(emit("sp"))
    es.close()
    stats = {e: len(Sd.ops[e]) for e in ENGS}
    return nc, stats


def make_consts():
    c = np.zeros((128, NCST), np.float32)
    i = np.arange(128)
    c[:, K_IDENT:K_IDENT + 128] = np.eye(128, dtype=np.float32)
    c[:, K_UINC:K_UINC + 128] = (i[:, None] >= i[None, :]).astype(np.float32)
    same = (i[:, None] // 64) == (i[None, :] // 64)
    c[:, K_TRI:K_TRI + 128] = ((i[:, None] <= i[None, :]) & same).astype(np.float32)
    c[:, K_SBM:K_SBM + 128] = (i[:, None] < i[None, :]).astype(np.float32)
    tt_ = np.arange(16)
    for g in range(4):
        w = 2 << g
        c[:, K_PRC + g * 16:K_PRC + (g + 1) * 16] = (1.0 / np.minimum(tt_ + 1, w)).astype(np.float32)[None, :]
    return c


def host_layout(inputs):
    f = lambda a: np.ascontiguousarray(np.asarray(a, dtype=np.float32))
    conv_w = f(inputs["conv_w"])
    pp = np.zeros((L, 128, NPP), np.float32)
    cw = conv_w.reshape(L, 3, 4, 128)
    pp[:, :, PP_CONV:PP_CONV + 12] = cw.transpose(0, 3, 2, 1).reshape(L, 128, 12)
    pp[:, :, PP_SBQ] = f(inputs["sb_q_g"])
    pp[:, :, PP_SBK] = f(inputs["sb_k_g"])
    pp[:, :, PP_GLN] = f(inputs["gla_norm_g"])
    pp[:, :, PP_PSC:PP_PSC + 4] = f(inputs["pool_scale"]).reshape(L, 4, 128).transpose(0, 2, 1)
    shared = {
        "w_in": f(inputs["w_in"]),
        "w_out": f(inputs["w_out"]),
        "w_gate": f(inputs["w_gate"]),
        "w_up": f(inputs["w_up"]),
        "w_down": f(inputs["w_down"]),
        "g1b": np.ascontiguousarray(np.broadcast_to(f(inputs["norm1_g"])[:, None, :], (L, 128, D))),
        "g2b": np.ascontiguousarray(np.broadcast_to(f(inputs["norm2_g"])[:, None, :], (L, 128, D))),
        "pp": pp,
        "aw": f(inputs["gla_a_w"]),
        "ab": f(inputs["gla_a_b"]).reshape(L, 1, 256),
        "pool_w": f(inputs["pool_w"]),
        "cst": make_consts(),
    }
    return shared


_CACHE = {}


def kernel(**inputs):
    x = np.ascontiguousarray(np.asarray(inputs["x"], dtype=np.float32))
    shared = host_layout(inputs)
    if "nc" not in _CACHE:
        _CACHE["nc"] = build_program(L)[0]
    nc = _CACHE["nc"]
    in_maps = [dict(shared, x=x[b]) for b in range(NCORES)]
    res = run_bass_kernel_spmd(nc, in_maps, core_ids=list(range(NCORES)))
    out = np.stack([np.asarray(r["y"], dtype=np.float32) for r in res.results], axis=0)
    return out
```

```python
import contextlib
from collections import deque

import numpy as np
import concourse.bass as bass
import concourse.mybir as mybir
from concourse.bass_utils import run_bass_kernel_spmd

F32 = mybir.dt.float32
BF16 = mybir.dt.bfloat16
AF = mybir.ActivationFunctionType
ALU = mybir.AluOpType

D = 2048
S = 2048
L = 4
NCORES = 8
INC = 5136
DFF = 5632
NFF = DFF // 128
TT = 512
NTILE = S // TT
EPS = 1e-6
NW = 4
SQRT128 = float(np.sqrt(128.0))
import os
SAME_ENGINE_SYNC = os.environ.get("K_SES", "1") == "1"

C_CONV = 0
C_SQ = 1536
C_SK = 2048
C_SV = 2560
C_GQ = 3072
C_GK = 3328
C_GV = 3584
C_GR = 4096
C_GA = 4608
C_PU = 4624

PP_CONV = 0
PP_SBQ = 12
PP_SBK = 13
PP_GLN = 14
PP_PSC = 15
NPP = 19

K_IDENT = 0
K_UINC = 128
K_TRI = 256
K_SBM = 384
K_PRC = 384 + 128
NCST = K_PRC + 64


ENGS = ("pe", "act", "dve", "pool", "sp")


class Op:
    __slots__ = ("eng", "fn", "deps", "dma", "needs_inc", "count", "semkey", "idx", "waits")


class Sched:
    def __init__(self):
        self.ops = {e: [] for e in ENGS}
        self.lastw = {}
        self.readers = {}
        self.last_by_key = {}

    def add(self, eng, fn, reads=(), writes=(), dma=None):
        op = Op()
        op.eng = eng
        op.fn = fn
        op.dma = dma
        op.semkey = ("dma", dma) if dma is not None else ("eng", eng)
        op.needs_inc = dma is not None
        op.count = None
        op.waits = None
        deps = set()
        for r in reads:
            w = self.lastw.get(r)
            if w is not None:
                deps.add(w)
        for wkey in writes:
            w = self.lastw.get(wkey)
            if w is not None:
                deps.add(w)
            rd = self.readers.get(wkey)
            if rd:
                deps.update(rd.values())
        op.deps = deps
        for r in reads:
            self.readers.setdefault(r, {})[op.semkey] = op
        for wkey in writes:
            self.lastw[wkey] = op
            self.readers[wkey] = {}
        op.idx = len(self.ops[eng])
        self.ops[eng].append(op)
        if fn is not None and dma is None:
            self.last_by_key[op.semkey] = op
        return op

    def fence(self):
        lasts = [self.last_by_key[("eng", e)] for e in ("pe", "act", "dve") if ("eng", e) in self.last_by_key]
        for e in ("pe", "act", "dve"):
            op = self.add(e, None)
            op.deps = set(lasts)

    def finalize(self):
        for e in ENGS:
            known = {}
            for op in self.ops[e]:
                need = {}
                for d in op.deps:
                    if d.dma is None and d.eng == e:
                        if e == "pe" or not SAME_ENGINE_SYNC:
                            continue
                    k = d.semkey
                    if known.get(k, -1) >= d.idx:
                        continue
                    if k not in need or need[k].idx < d.idx:
                        need[k] = d
                for k, d in need.items():
                    known[k] = d.idx
                    d.needs_inc = True
                op.waits = list(need.values())
        counters = {}
        for e in ENGS:
            for op in self.ops[e]:
                if op.needs_inc and op.fn is not None:
                    k = op.semkey
                    counters[k] = counters.get(k, 0) + (16 if op.dma is not None else 1)
                    op.count = counters[k]
        return counters


def build_program(n_layers=L, dbg=None, dbg_tile=0):
    nc = bass.Bass("TRN2", target_bir_lowering=False)
    dbg = dbg or {}

    def din(name, shape):
        return nc.dram_tensor(name, list(shape), F32, kind="ExternalInput").ap()

    x_in = din("x", (S, D))
    w_in = din("w_in", (L, D, INC))
    w_out = din("w_out", (L, D, D))
    w_gate = din("w_gate", (L, D, DFF))
    w_up = din("w_up", (L, D, DFF))
    w_down = din("w_down", (L, DFF, D))
    g1b_d = din("g1b", (L, 128, D))
    g2b_d = din("g2b", (L, 128, D))
    pp_d = din("pp", (L, 128, NPP))
    aw_d = din("aw", (L, 16, 256))
    ab_d = din("ab", (L, 1, 256))
    pw_d = din("pool_w", (L, 4, 128, 128))
    cst_d = din("cst", (128, NCST))
    y_out = nc.dram_tensor("y", [S, D], F32, kind="ExternalOutput").ap()
    xs = nc.dram_tensor("xs", [S, D], F32, kind="Internal").ap()
    dbg_out = {}
    for name, shape in dbg.items():
        dbg_out[name] = nc.dram_tensor("dbg_" + name, list(shape), F32, kind="ExternalOutput").ap()

    Sd = Sched()
    es = contextlib.ExitStack()

    def sb(name, shape, dt):
        return es.enter_context(nc.sbuf_tensor("sb_" + name, list(shape), dt))

    cst = sb("cst", (128, NCST), F32)
    identb = sb("identb", (128, 128), BF16)
    onesb = sb("onesb", (128, 128), BF16)
    onesf = sb("onesf", (128, 128), F32)
    uincb = sb("uincb", (128, 128), BF16)
    gbuf = [sb(f"gb{i}", (128, D), F32) for i in range(2)]
    pp = sb("pp", (128, NPP), F32)
    aw = sb("aw", (16, 256), F32)
    ab = sb("ab", (1, 256), F32)
    pwb = sb("pwb", (128, 4, 128), BF16)
    hT = sb("hT", (128, 16, TT), BF16)
    wslots = [sb(f"wslot{i}", (128, 16 * 256), BF16) for i in range(NW)]
    xring = [sb(f"xring{i}", (128, D), F32) for i in range(2)]
    hbring = [sb(f"hbring{i}", (128, D), BF16) for i in range(2)]
    stat = sb("stat", (128, 64), F32)
    NXO = 4
    xo = [sb(f"xo{i}", (128, 512), F32) for i in range(NXO)]
    ccar = sb("ccar", (128, 4, 2), F32)
    pcar = sb("pcar", (128, 4, 16), F32)
    dbgbuf = [sb(f"dbgbuf{i}", (128, 512), F32) for i in range(1)] if dbg else None

    UN = 52224
    U = sb("U", (128, UN), BF16)
    ucur = [0]

    def carve(nelem, dt):
        n16 = nelem * (2 if dt == F32 else 1)
        a = ucur[0]
        assert a + n16 <= UN, f"union overflow {a + n16} > {UN}"
        ucur[0] = a + n16
        v = U[:, a:a + n16]
        if dt == F32:
            v = v.bitcast(F32)
        return v

    banks = [es.enter_context(nc.psum_tensor(f"bank{i}", [128, 512], F32)) for i in range(8)]
    freeb = deque(range(8))

    def getbank():
        return freeb.popleft()

    def relbank(b):
        freeb.append(b)

    def BK(b):
        return ("B", b)

    def A(eng, fn, reads=(), writes=(), dma=None):
        return Sd.add(eng, fn, reads, writes, dma)

    def mm(out, lhsT, rhs, start, stop):
        return lambda e: e.matmul(out, lhsT, rhs, start=start, stop=stop)

    def act(out, in_, func, **kw):
        return lambda e: e.activation(out=out, in_=in_, func=func, **kw)

    def tcopy(out, in_):
        return lambda e: e.tensor_copy(out=out, in_=in_)

    def tt(out, in0, in1, op):
        return lambda e: e.tensor_tensor(out=out, in0=in0, in1=in1, op=op)

    def ts(out, in0, s1, s2, op0, op1=None):
        if op1 is None:
            return lambda e: e.tensor_scalar(out=out, in0=in0, scalar1=s1, scalar2=None, op0=op0)
        return lambda e: e.tensor_scalar(out=out, in0=in0, scalar1=s1, scalar2=s2, op0=op0, op1=op1)

    def stt(out, in0, scalar, in1, op0, op1):
        return lambda e: e.scalar_tensor_tensor(out=out, in0=in0, scalar=scalar, in1=in1, op0=op0, op1=op1)

    def dma(out, in_):
        return lambda e: e.dma_start(out=out, in_=in_)

    def memset(ap, v):
        return lambda e: e.memset(ap, v)

    dbg_n = [0]

    def dump(name, src_ap, reads, rows=None):
        if name not in dbg_out:
            return
        dst = dbg_out[name]
        dbg_n[0] += 1
        A("sp", dma(dst if rows is None else dst[rows], src_ap), reads=reads, writes=[("dbg", name, dbg_n[0])],
          dma=f"dbg{dbg_n[0] % 4}")

    stat_i = [0]

    def statcol():
        c = stat_i[0] % 64
        stat_i[0] += 1
        return c

    A("sp", dma(cst[:], cst_d[:, :]), writes=["cst"], dma="c0")
    A("dve", tcopy(identb[:], cst[:, K_IDENT:K_IDENT + 128]), reads=["cst"], writes=["identb"])
    A("dve", tcopy(uincb[:], cst[:, K_UINC:K_UINC + 128]), reads=["cst"], writes=["uincb"])
    A("dve", memset(onesb[:], 1.0), writes=["onesb"])
    A("dve", memset(onesf[:], 1.0), writes=["onesf"])
    A("dve", memset(stat[:], 12345.0), writes=[("stat", c) for c in range(64)])

    wn = [0]

    def wload(src3, nk, ncol):
        s = wn[0] % NW
        wn[0] += 1
        view = wslots[s][:, 0:nk * ncol].rearrange("p (k n) -> p k n", k=nk)
        A("pool", dma(view, src3), writes=[("w", s)], dma=f"w{s}")
        return view, ("w", s)

    def wsrc(wl, r0, nk, c0, ncol):
        return wl[r0:r0 + nk * 128, c0:c0 + ncol].rearrange("(k p) n -> p k n", p=128)

    HT_ALL = [("hT", tb) for tb in range(4)]

    def fm_group(wt, wkey, off, M, nk=16, rhs_of=None, rhs_keys=None):
        b = getbank()
        for kc in range(nk):
            rhs = hT[:, kc, :] if rhs_of is None else rhs_of(kc)
            A("pe", mm(banks[b][0:M, :], wt[:, kc, off:off + M], rhs, kc == 0, kc == nk - 1),
              reads=[wkey] + (HT_ALL if rhs_keys is None else rhs_keys), writes=[BK(b)])
        return b

    def tm_group(wt, wkey, tb, ncol):
        b = getbank()
        for kc in range(16):
            A("pe", mm(banks[b][:, 0:ncol], hT[:, kc, tb * 128:(tb + 1) * 128], wt[:, kc, 0:ncol], kc == 0, kc == 15),
              reads=[wkey, ("hT", tb)], writes=[BK(b)])
        return b

    xn = [0]
    evac_flip = [0]

    norm_slot = {}

    def norm_pre(src, srcname, ti, tb, gb, gbkey):
        r = ti * 4 + tb
        sl = xn[0] % 2
        xn[0] += 1
        norm_slot[tb] = sl
        xk = ("xring", sl)
        hk = ("hbring", sl)
        A("sp", dma(xring[sl][:], src[r * 128:(r + 1) * 128, :]),
          reads=[(srcname, r, n) for n in range(4)], writes=[xk], dma=f"xl{sl}")
        c = statcol()
        A("act", act(hbring[sl][:], xring[sl][:], AF.Square, accum_out=stat[:, c:c + 1]),
          reads=[xk], writes=[hk, ("stat", c)])
        A("act", act(stat[:, c:c + 1], stat[:, c:c + 1], AF.Ln, scale=1.0 / D, bias=EPS),
          reads=[("stat", c)], writes=[("stat", c)])
        A("act", act(stat[:, c:c + 1], stat[:, c:c + 1], AF.Exp, scale=-0.5),
          reads=[("stat", c)], writes=[("stat", c)])
        A("dve", stt(hbring[sl][:], xring[sl][:], stat[:, c:c + 1], gb[:], ALU.mult, ALU.mult),
          reads=[xk, ("stat", c), gbkey], writes=[hk])

    def norm_post(tb):
        sl = norm_slot[tb]
        hk = ("hbring", sl)
        for half in range(2):
            b = getbank()
            bv = banks[b][:].bitcast(BF16)
            for k8 in range(8):
                kc = half * 8 + k8
                A("pe", lambda e, o=bv[:, k8 * 128:(k8 + 1) * 128], i=hbring[sl][:, kc * 128:(kc + 1) * 128]:
                  e.transpose(out=o, in_=i, identity=identb[:]),
                  reads=[hk, "identb"], writes=[BK(b)])
            src_v = bv[:, 0:1024].rearrange("p (k t) -> p k t", k=8)
            dst_v = hT[:, half * 8:(half + 1) * 8, tb * 128:(tb + 1) * 128]
            eng = "act" if (evac_flip[0] % 2 == 0) else "dve"
            evac_flip[0] += 1
            if eng == "act":
                A("act", act(dst_v, src_v, AF.Copy), reads=[BK(b)], writes=[("hT", tb)])
            else:
                A("dve", tcopy(dst_v, src_v), reads=[BK(b)], writes=[("hT", tb)])
            relbank(b)

    xon = [0]

    def out_proj(ti, wl, nkc, actT, actkeys_of, src, srcname, dst, dstname, pre=None, post=None):
        ktiles = [(k0, min(8, nkc - k0)) for k0 in range(0, nkc, 8)]
        for ng in range(4):
            xslots = []
            for tb in range(4):
                r = ti * 4 + tb
                s = xon[0] % NXO
                xon[0] += 1
                xslots.append(s)
                A("act", dma(xo[s][:], src[r * 128:(r + 1) * 128, ng * 512:(ng + 1) * 512]),
                  reads=[(srcname, r, ng)], writes=[("xo", s)], dma=f"xo{s}")
            if pre is not None:
                pre(ng)
            bs = [getbank() for _ in range(4)]
            for (k0, nk) in ktiles:
                wt, wkey = wload(wsrc(wl, k0 * 128, nk, ng * 512, 512), nk, 512)
                for tb in range(4):
                    for k8 in range(nk):
                        kc = k0 + k8
                        A("pe", mm(banks[bs[tb]][:, :], actT[:, kc, tb * 128:(tb + 1) * 128], wt[:, k8, :],
                                   kc == 0, kc == nkc - 1),
                          reads=[wkey] + actkeys_of(kc), writes=[BK(bs[tb])])
            if post is not None and ng > 0:
                post(ng - 1)
            for tb in range(4):
                r = ti * 4 + tb
                s = xslots[tb]
                A("dve", tt(xo[s][:], xo[s][:], banks[bs[tb]][:, :], ALU.add),
                  reads=[("xo", s), BK(bs[tb])], writes=[("xo", s)])
                A("sp", dma(dst[r * 128:(r + 1) * 128, ng * 512:(ng + 1) * 512], xo[s][:]),
                  reads=[("xo", s)], writes=[(dstname, r, ng)], dma=f"xs{s}")
                relbank(bs[tb])
        if post is not None:
            post(3)

    ucur[0] = 0
    mixedT = carve(16 * TT, BF16).rearrange("p (c t) -> p c t", c=16)
    kTc = carve(4 * S, BF16).rearrange("p (h t) -> p h t", h=4)
    Vc = carve(16 * 512, BF16).rearrange("p (b c) -> p b c", b=16)
    gstate = carve(2 * 256, F32).rearrange("p (c v) -> p c v", c=2)
    gstate_b = carve(2 * 256, BF16).rearrange("p (c v) -> p c v", c=2)
    a_mix_base = ucur[0]
    ubuf = carve(4 * 514, F32).rearrange("p (c t) -> p c t", c=4)
    ybuf = carve(4 * 512, F32).rearrange("p (c t) -> p c t", c=4)
    cbuf = carve(4 * 512, F32).rearrange("p (c t) -> p c t", c=4)
    conv_end = ucur[0]
    ubp = carve(4 * 528, F32).rearrange("p (c t) -> p c t", c=4)
    plev = [carve(528, F32) for _ in range(4)]
    pooled_f = [carve(512, F32) for _ in range(2)]
    pooled_b = [carve(512, BF16) for _ in range(2)]
    pool_end = ucur[0]
    ucur[0] = a_mix_base
    qT = carve(4 * 512, BF16).rearrange("p (h t) -> p h t", h=4)
    qf = [carve(512, F32) for _ in range(2)]
    sqb = [carve(512, BF16) for _ in range(2)]
    rsb = [carve(512, F32) for _ in range(2)]
    NEB = 4
    Eb = [carve(512, F32) for _ in range(NEB)]
    spb = [carve(512, BF16) for _ in range(NEB)]
    cumb = [carve(512, F32) for _ in range(2)]
    Xb = [carve(512, F32) for _ in range(2)]
    Ab = [carve(512, BF16) for _ in range(NEB)]
    totb = carve(512, F32)
    sb_end = ucur[0]
    ucur[0] = a_mix_base
    gqT = carve(2 * 512, F32).rearrange("p (c t) -> p c t", c=2)
    gkT = carve(2 * 512, F32).rearrange("p (c t) -> p c t", c=2)
    gktm = carve(4 * 256, F32).rearrange("p (b c) -> p b c", b=4)
    gv = carve(4 * 512, BF16).rearrange("p (b c) -> p b c", b=4)
    srT = carve(4 * 512, F32).rearrange("p (c t) -> p c t", c=4)
    alr = carve(512, F32)
    oT = carve(4 * 512, F32).rearrange("p (h t) -> p h t", h=4)
    g_e1 = [carve(256, F32) for _ in range(2)]
    g_gp = [carve(256, F32) for _ in range(2)]
    g_eb = [carve(256, F32).rearrange("p (c t) -> p c t", c=2) for _ in range(2)]
    g_enb = [carve(256, F32).rearrange("p (c t) -> p c t", c=2) for _ in range(2)]
    g_enbtm = [carve(256, F32) for _ in range(2)]
    g_qe = [carve(256, BF16).rearrange("p (c t) -> p c t", c=2) for _ in range(2)]
    g_ke = [carve(256, BF16).rearrange("p (c t) -> p c t", c=2) for _ in range(2)]
    g_ketm = [carve(256, BF16) for _ in range(2)]
    g_att = [carve(128, BF16) for _ in range(4)]
    g_sq = [carve(512, BF16) for _ in range(1)]
    g_rn = [carve(512, F32) for _ in range(1)]
    gla_end = ucur[0]
    a_end = max(conv_end, pool_end, sb_end, gla_end)
    ucur[0] = 0
    actT = carve(NFF * TT, BF16).rearrange("p (c t) -> p c t", c=NFF)
    sgb = [carve(512, F32) for _ in range(4)]
    b_end = ucur[0]
    assert max(a_end, b_end) <= UN

    w_tri = cst[:, K_TRI:K_TRI + 128]

    def sweep_a(l, ti, src, srcname, dst, dstname, pre, post):
        t0 = ti * TT
        wl = w_in[l]

        if ti == 0:
            A("dve", memset(ccar[:], 0.0), writes=["ccar"])
        A("dve", tcopy(ubuf[:, :, 0:2], ccar[:]), reads=["ccar"], writes=[("ubuf", c) for c in range(4)])
        for t in (2, 3, 4, 5, 0, 1):
            wt, wkey = wload(wsrc(wl, 0, 16, C_CONV + t * 256, 256), 16, 256)
            for j in range(2):
                c = (t % 2) * 2 + j
                b = fm_group(wt, wkey, j * 128, 128)
                if 2 <= t < 4:
                    A("act", act(cbuf[:, c, :], banks[b][:, :], AF.Copy), reads=[BK(b)], writes=[("cbuf", c)])
                elif t >= 4:
                    yt = ybuf[:, c, :]
                    yk = ("ybuf", c)
                    A("dve", tt(ubuf[:, c, 2:514], cbuf[:, c, :], banks[b][:, :], ALU.mult),
                      reads=[("cbuf", c), BK(b)], writes=[("ubuf", c)])
                    A("dve", ts(yt, ubuf[:, c, 2:514], pp[:, PP_CONV + c * 3 + 2:PP_CONV + c * 3 + 3], None, ALU.mult),
                      reads=[("ubuf", c), "pp"], writes=[yk])
                    A("dve", stt(yt, ubuf[:, c, 1:513], pp[:, PP_CONV + c * 3 + 1:PP_CONV + c * 3 + 2], yt,
                                 ALU.mult, ALU.add), reads=[("ubuf", c), "pp", yk], writes=[yk])
                    A("dve", stt(yt, ubuf[:, c, 0:512], pp[:, PP_CONV + c * 3 + 0:PP_CONV + c * 3 + 1], yt,
                                 ALU.mult, ALU.add), reads=[("ubuf", c), "pp", yk], writes=[yk])
                    A("dve", tcopy(ccar[:, c, :], ubuf[:, c, 512:514]), reads=[("ubuf", c)], writes=["ccar"])
                else:
                    A("dve", tt(mixedT[:, c, :], ybuf[:, c, :], banks[b][:, :], ALU.mult),
                      reads=[("ybuf", c), BK(b)], writes=[("mixedT", c)])
                relbank(b)

        if ti == 0:
            A("dve", memset(pcar[:], 0.0), writes=["pcar"])
        A("dve", tcopy(ubp[:, :, 0:16], pcar[:]), reads=["pcar"], writes=[("ubp", g) for g in range(4)])
        for t in range(2):
            wt, wkey = wload(wsrc(wl, 0, 16, C_PU + t * 256, 256), 16, 256)
            for j in range(2):
                g = t * 2 + j
                win = 2 << g
                b = fm_group(wt, wkey, j * 128, 128)
                A("act", act(ubp[:, g, 16:528], banks[b][:, :], AF.Copy), reads=[BK(b)], writes=[("ubp", g)])
                relbank(b)
        for t in range(2):
            for j in range(2):
                g = t * 2 + j
                win = 2 << g
                prev = ubp[:, g, :]
                prevk = ("ubp", g)
                sh = 1
                lo = 1
                for lev in range(g + 1):
                    cur = plev[lev]
                    A("dve", tt(cur[:, lo:528], prev[:, lo:528], prev[:, lo - sh:528 - sh], ALU.add),
                      reads=[prevk], writes=[("plev", lev)])
                    prev = cur
                    prevk = ("plev", lev)
                    sh *= 2
                    lo += sh
                pf = pooled_f[g % 2]
                pb_ = pooled_b[g % 2]
                A("dve", stt(pf, prev[:, 16:528], 1.0 / win, ubp[:, g, 16:528], ALU.mult, ALU.subtract),
                  reads=[prevk, ("ubp", g)], writes=[("pooled_f", g % 2)])
                if ti == 0:
                    A("dve", tt(pf[:, 0:16], prev[:, 16:32], cst[:, K_PRC + g * 16:K_PRC + (g + 1) * 16], ALU.mult),
                      reads=[prevk, "cst", ("pooled_f", g % 2)], writes=[("pooled_f", g % 2)])
                    A("dve", tt(pf[:, 0:16], pf[:, 0:16], ubp[:, g, 16:32], ALU.subtract),
                      reads=[("ubp", g), ("pooled_f", g % 2)], writes=[("pooled_f", g % 2)])
                A("dve", tcopy(pb_, pf), reads=[("pooled_f", g % 2)], writes=[("pooled_b", g % 2)])
                A("dve", tcopy(pcar[:, g, :], ubp[:, g, 512:528]), reads=[("ubp", g)], writes=["pcar"])
                b2 = getbank()
                A("pe", mm(banks[b2][:, :], pwb[:, g, :], pb_, True, True),
                  reads=["pwb", ("pooled_b", g % 2)], writes=[BK(b2)])
                A("act", act(mixedT[:, 12 + g, :], banks[b2][:, :], AF.Copy, scale=pp[:, PP_PSC + g:PP_PSC + g + 1]),
                  reads=[BK(b2), "pp"], writes=[("mixedT", 12 + g)])
                relbank(b2)
        Sd.fence()

        qi = 0
        pending = [None]

        def sb_norm_chain(s2, isq, h):
            def run():
                b2 = getbank()
                A("pe", mm(banks[b2][:, :], onesb[:], sqb[s2], True, True), reads=["onesb", ("sqb", s2)], writes=[BK(b2)])
                A("act", act(rsb[s2], banks[b2][:, :], AF.Ln, bias=128.0 * EPS), reads=[BK(b2)], writes=[("rsb", s2)])
                relbank(b2)
                A("act", act(rsb[s2], rsb[s2], AF.Exp, scale=-0.5), reads=[("rsb", s2)], writes=[("rsb", s2)])
                gcol = PP_SBQ if isq else PP_SBK
                if isq:
                    dstv = qT[:, h, :]
                    dk = ("qT", h)
                else:
                    dstv = kTc[:, h, t0:t0 + TT]
                    dk = ("kT", h, ti)
                A("dve", stt(dstv, qf[s2], pp[:, gcol:gcol + 1], rsb[s2], ALU.mult, ALU.mult),
                  reads=[("qf", s2), "pp", ("rsb", s2)], writes=[dk])
            return run

        for t in range(4):
            wt, wkey = wload(wsrc(wl, 0, 16, C_SQ + t * 256, 256), 16, 256)
            for j in range(2):
                h = (t % 2) * 2 + j
                isq = t < 2
                b = fm_group(wt, wkey, j * 128, 128)
                s2 = qi % 2
                qi += 1
                A("act", act(qf[s2], banks[b][:, :], AF.Copy), reads=[BK(b)], writes=[("qf", s2)])
                A("act", act(sqb[s2], banks[b][:, :], AF.Square), reads=[BK(b)], writes=[("sqb", s2)])
                relbank(b)
                if pending[0] is not None:
                    pending[0]()
                pending[0] = sb_norm_chain(s2, isq, h)
        sv_first = True
        for t in range(2):
            wt, wkey = wload(wsrc(wl, 0, 16, C_SV + t * 256, 256), 16, 256)
            for tb in range(4):
                b = tm_group(wt, wkey, tb, 256)
                A("act", act(Vc[:, ti * 4 + tb, t * 256:(t + 1) * 256], banks[b][:, 0:256], AF.Copy),
                  reads=[BK(b)], writes=[("Vc", ti * 4 + tb, t)])
                relbank(b)
                if pending[0] is not None:
                    pending[0]()
                    pending[0] = None
        pairs = [(h, kb) for h in range(4) for kb in range(ti * 4 + 3, -1, -1)]
        NP = len(pairs)
        st = {}
        po_bank = {}

        def stage_a(n):
            h, kb = pairs[n]
            j = kb - ti * 4
            q0 = max(j, 0) * 128
            e = n % NEB
            bz = getbank()
            A("pe", mm(banks[bz][:, q0:512], kTc[:, h, kb * 128:(kb + 1) * 128], qT[:, h, q0:512], True, True),
              reads=[("kT", h, kb // 4), ("qT", h)], writes=[BK(bz)])
            A("act", act(Eb[e][:, q0:512], banks[bz][:, q0:512], AF.Exp, scale=SQRT128), reads=[BK(bz)], writes=[("E", e)])
            relbank(bz)
            if j >= 0:
                A("dve", tt(Eb[e][:, q0:q0 + 128], Eb[e][:, q0:q0 + 128], cst[:, K_SBM:K_SBM + 128], ALU.mult),
                  reads=[("E", e), "cst"], writes=[("E", e)])
            A("act", act(spb[e][:, q0:512], Eb[e][:, q0:512], AF.Ln, bias=1.0), reads=[("E", e)], writes=[("sp", e)])

        def stage_b(n):
            h, kb = pairs[n]
            j = kb - ti * 4
            q0 = max(j, 0) * 128
            e = n % NEB
            first = kb == ti * 4 + 3
            last = kb == 0
            if first:
                A("dve", memset(totb, 0.0), writes=["tot"])
            bc = getbank()
            A("pe", mm(banks[bc][:, q0:512], uincb[:], spb[e][:, q0:512], True, True), reads=["uincb", ("sp", e)], writes=[BK(bc)])
            c2 = n % 2
            A("dve", tt(cumb[c2][:, q0:512], banks[bc][:, q0:512], totb[:, q0:512], ALU.add),
              reads=[BK(bc), "tot"], writes=[("cum", c2)])
            relbank(bc)
            if not last:
                bo = getbank()
                A("pe", mm(banks[bo][:, q0:512], onesb[:], spb[e][:, q0:512], True, True), reads=["onesb", ("sp", e)], writes=[BK(bo)])
                A("dve", tt(totb[:, q0:512], totb[:, q0:512], banks[bo][:, q0:512], ALU.add), reads=[BK(bo), "tot"], writes=["tot"])
                relbank(bo)
            A("act", act(Xb[c2][:, q0:512], cumb[c2][:, q0:512], AF.Exp, scale=-1.0), reads=[("cum", c2)], writes=[("X", c2)])
            if q0 > 0:
                A("pool", memset(Ab[e][:, 0:q0], 0.0), writes=[("A", e)])
            A("pool", tt(Ab[e][:, q0:512], Eb[e][:, q0:512], Xb[c2][:, q0:512], ALU.mult), reads=[("E", e), ("X", c2)], writes=[("A", e)])

        def stage_c(n):
            h, kb = pairs[n]
            e = n % NEB
            first = kb == ti * 4 + 3
            last = kb == 0
            if first:
                po_bank[h] = getbank()
            bp = po_bank[h]
            A("pe", mm(banks[bp][:, :], Vc[:, kb, h * 128:(h + 1) * 128], Ab[e], first, last),
              reads=[("Vc", kb, h // 2), ("A", e)], writes=[BK(bp)])
            if last:
                A("act", act(mixedT[:, 4 + h, :], banks[bp][:, :], AF.Copy), reads=[BK(bp)], writes=[("mixedT", 4 + h)])
                relbank(bp)

        gla_pref = [wload(wsrc(wl, 0, 16, C_GQ, 256), 16, 256),
                    wload(wsrc(wl, 0, 16, C_GK, 256), 16, 256),
                    wload(wsrc(wl, 0, 16, C_GV, 256), 16, 256)]
        for n in range(NP + 3):
            if n < NP:
                stage_a(n)
            if 0 <= n - 1 < NP:
                stage_b(n - 1)
            if 0 <= n - 3 < NP:
                stage_c(n - 3)
        Sd.fence()

        if ti == 0:
            A("dve", memset(gstate[:], 0.0), writes=[("gstate", c) for c in range(2)])
            A("dve", memset(gstate_b[:], 0.0), writes=[("gstate_b", c) for c in range(2)])
        wt, wkey = gla_pref[0]
        for c in range(2):
            b = fm_group(wt, wkey, c * 128, 128)
            A("act", act(gqT[:, c, :], banks[b][:, :], AF.Copy), reads=[BK(b)], writes=[("gqT", c)])
            relbank(b)
        wt, wkey = gla_pref[1]
        for c in range(2):
            b = fm_group(wt, wkey, c * 128, 128)
            A("act", act(gkT[:, c, :], banks[b][:, :], AF.Copy), reads=[BK(b)], writes=[("gkT", c)])
            relbank(b)
        for tb in range(4):
            b = tm_group(wt, wkey, tb, 256)
            A("act", act(gktm[:, tb, :], banks[b][:, 0:256], AF.Copy), reads=[BK(b)], writes=[("gktm", tb)])
            relbank(b)
        for t in range(2):
            wt, wkey = gla_pref[2] if t == 0 else wload(wsrc(wl, 0, 16, C_GV + t * 256, 256), 16, 256)
            for tb in range(4):
                b = tm_group(wt, wkey, tb, 256)
                A("act", act(gv[:, tb, t * 256:(t + 1) * 256], banks[b][:, 0:256], AF.Copy),
                  reads=[BK(b)], writes=[("gv", tb, t)])
                relbank(b)
        for t in range(2):
            wt, wkey = wload(wsrc(wl, 0, 16, C_GR + t * 256, 256), 16, 256)
            for j in range(2):
                c = t * 2 + j
                b = fm_group(wt, wkey, j * 128, 128)
                A("act", act(srT[:, c, :], banks[b][:, :], AF.Silu), reads=[BK(b)], writes=[("srT", c)])
                relbank(b)
        wt, wkey = wload(wsrc(wl, 0, 16, C_GA, 16), 16, 16)
        b = fm_group(wt, wkey, 0, 16)
        A("act", act(alr[0:16, :], banks[b][0:16, :], AF.Copy), reads=[BK(b)], writes=["alr"])
        relbank(b)

        def gla_G(tb):
            tsl = slice(tb * 128, (tb + 1) * 128)
            s2 = tb % 2
            e1, gp, eb, enb, enbtm = g_e1[s2], g_gp[s2], g_eb[s2], g_enb[s2], g_enbtm[s2]
            bg = getbank()
            A("pe", mm(banks[bg][:, 0:256], alr[0:16, tsl], aw[0:16, :], True, False), reads=["alr", "aw"], writes=[BK(bg)])
            A("pe", mm(banks[bg][:, 0:256], onesf[0:1, 0:128], ab[0:1, :], False, True), reads=["onesf", "ab"], writes=[BK(bg)])
            A("act", act(e1, banks[bg][:, 0:256], AF.Exp, scale=-1.0), reads=[BK(bg)], writes=[("g_e1", s2)])
            relbank(bg)
            A("act", act(gp, e1, AF.Ln, bias=1.0), reads=[("g_e1", s2)], writes=[("g_gp", s2)])
            bfm = getbank()
            for c in range(2):
                A("pe", mm(banks[bfm][:, c * 128:(c + 1) * 128], gp[:, c * 128:(c + 1) * 128], w_tri, True, True),
                  reads=[("g_gp", s2), "cst"], writes=[BK(bfm)])
            btm = getbank()
            A("pe", mm(banks[btm][:, 0:256], w_tri, gp, True, True), reads=[("g_gp", s2), "cst"], writes=[BK(btm)])
            pfm = banks[bfm][:, 0:256].rearrange("p (c t) -> p c t", c=2)
            A("act", act(eb, pfm, AF.Exp, scale=-1.0 / 16.0), reads=[BK(bfm)], writes=[("g_eb", s2)])
            A("act", act(enb, pfm, AF.Exp, scale=1.0 / 16.0), reads=[BK(bfm)], writes=[("g_enb", s2)])
            relbank(bfm)
            A("act", act(enbtm, banks[btm][:, 0:256], AF.Exp, scale=1.0 / 16.0), reads=[BK(btm)], writes=[("g_enbtm", s2)])
            relbank(btm)
            qe = g_qe[s2]
            ke = g_ke[s2]
            ketm = g_ketm[s2]
            A("dve", stt(qe, gqT[:, :, tsl], 0.125, eb, ALU.mult, ALU.mult),
              reads=[("gqT", 0), ("gqT", 1), ("g_eb", s2)], writes=[("g_qe", s2)])
            A("dve", tt(ke, gkT[:, :, tsl], enb, ALU.mult), reads=[("gkT", 0), ("gkT", 1), ("g_enb", s2)], writes=[("g_ke", s2)])
            A("dve", tt(ketm, gktm[:, tb, :], enbtm, ALU.mult), reads=[("gktm", tb), ("g_enbtm", s2)], writes=[("g_ketm", s2)])

        def gla_R(tb):
            tsl = slice(tb * 128, (tb + 1) * 128)
            s2 = tb % 2
            eb = g_eb[s2]
            qe = g_qe[s2]
            ke = g_ke[s2]
            ketm = g_ketm[s2]
            po_b = {}
            for h in range(4):
                c = h // 2
                p0 = (h % 2) * 64
                ba = getbank()
                A("pe", mm(banks[ba][:, 0:128], ke[p0:p0 + 64, c, :], qe[p0:p0 + 64, c, :], True, True),
                  reads=[("g_ke", s2), ("g_qe", s2)], writes=[BK(ba)])
                a3 = h
                A("dve", tt(g_att[a3], banks[ba][:, 0:128], w_tri, ALU.mult), reads=[BK(ba), "cst"], writes=[("g_att", a3)])
                relbank(ba)
                po_b[h] = (getbank(), a3)
            for ch in range(2):
                csl = slice(ch * 64, (ch + 1) * 64)
                for h in range(4):
                    c = h // 2
                    p0 = (h % 2) * 64
                    bo, a3 = po_b[h]
                    A("pe", lambda e, o=banks[bo][:, ch * 64:(ch + 1) * 64],
                      l_=gstate_b[p0:p0 + 64, c, (h % 2) * 128:(h % 2 + 1) * 128], r_=qe[p0:p0 + 64, c, csl], st_=(ch == 0):
                      e.matmul(o, l_, r_, start=st_, stop=False, skip_group_check=True),
                      reads=[("gstate_b", c), ("g_qe", s2)], writes=[BK(bo)])
                for c in range(2):
                    bs_ = getbank()
                    A("pe", mm(banks[bs_][:, 0:256], ketm[csl, c * 128:(c + 1) * 128], gv[csl, tb, c * 256:(c + 1) * 256],
                               True, True),
                      reads=[("g_ketm", s2), ("gv", tb, c)], writes=[BK(bs_)])
                    A("dve", tt(gstate[:, c, :], gstate[:, c, :], banks[bs_][:, 0:256], ALU.add),
                      reads=[("gstate", c), BK(bs_)], writes=[("gstate", c)])
                    relbank(bs_)
                    A("dve", ts(gstate[:, c, :], gstate[:, c, :], eb[:, c, ch * 64 + 63:ch * 64 + 64], None, ALU.mult),
                      reads=[("gstate", c), ("g_eb", s2)], writes=[("gstate", c)])
                    A("dve", tcopy(gstate_b[:, c, :], gstate[:, c, :]), reads=[("gstate", c)], writes=[("gstate_b", c)])
            for h in range(4):
                bo, a3 = po_b[h]
                A("pe", mm(banks[bo][:, 0:128], gv[:, tb, h * 128:(h + 1) * 128], g_att[a3], False, True),
                  reads=[("gv", tb, h // 2), ("g_att", a3)], writes=[BK(bo)])
                A("act", act(oT[:, h, tsl], banks[bo][:, 0:128], AF.Copy), reads=[BK(bo)], writes=[("oT", h)])
                relbank(bo)

        gla_G(0)
        gla_G(1)
        gla_R(0)
        gla_G(2)
        gla_R(1)
        gla_G(3)
        gla_R(2)
        gla_R(3)
        for h in range(4):
            s2 = 0
            A("act", act(g_sq[s2], oT[:, h, :], AF.Square), reads=[("oT", h)], writes=[("g_sq", s2)])
            bn = getbank()
            A("pe", mm(banks[bn][:, :], onesb[:], g_sq[s2], True, True), reads=["onesb", ("g_sq", s2)], writes=[BK(bn)])
            A("act", act(g_rn[s2], banks[bn][:, :], AF.Ln, bias=128.0 * EPS), reads=[BK(bn)], writes=[("g_rn", s2)])
            relbank(bn)
            A("act", act(g_rn[s2], g_rn[s2], AF.Exp, scale=-0.5), reads=[("g_rn", s2)], writes=[("g_rn", s2)])
            A("dve", stt(oT[:, h, :], oT[:, h, :], pp[:, PP_GLN:PP_GLN + 1], g_rn[s2], ALU.mult, ALU.mult),
              reads=[("oT", h), "pp", ("g_rn", s2)], writes=[("oT", h)])
            A("dve", stt(mixedT[:, 8 + h, :], oT[:, h, :], SQRT128, srT[:, h, :], ALU.mult, ALU.mult),
              reads=[("oT", h), ("srT", h)], writes=[("mixedT", 8 + h)])
        Sd.fence()

        if "mixedT" in dbg_out and ti == dbg_tile and l == 0:
            for c in range(16):
                s = 0
                A("dve", tcopy(dbgbuf[s][:], mixedT[:, c, :]), reads=[("mixedT", c)], writes=[("dbgbuf", s)])
                dump("mixedT", dbgbuf[s][:], [("dbgbuf", s)], rows=(slice(c * 128, (c + 1) * 128), slice(None)))
            Sd.fence()

        out_proj(ti, w_out[l], 16, mixedT, lambda kc: [("mixedT", kc)], src, srcname, dst, dstname, pre, post)
        Sd.fence()

    def sweep_b(l, ti, src, srcname, dst, dstname, pre, post):
        si = 0
        for grp in range(NFF // 4):
            bsets = []
            for wmat in (w_gate[l], w_up[l]):
                bset = [getbank() for _ in range(4)]
                bsets.append(bset)
                for kh in range(2):
                    wt, wk = wload(wsrc(wmat, kh * 1024, 8, grp * 512, 512), 8, 512)
                    for c in range(4):
                        for k8 in range(8):
                            kc = kh * 8 + k8
                            A("pe", mm(banks[bset[c]][:, :], wt[:, k8, c * 128:(c + 1) * 128], hT[:, kc, :],
                                       kc == 0, kc == 15),
                              reads=[wk] + HT_ALL, writes=[BK(bset[c])])
            slots = []
            for c in range(4):
                s4 = si % 4
                si += 1
                slots.append(s4)
                A("act", act(sgb[s4], banks[bsets[0][c]][:, :], AF.Silu), reads=[BK(bsets[0][c])], writes=[("sgb", s4)])
                relbank(bsets[0][c])
            for c in range(4):
                f = grp * 4 + c
                s4 = slots[c]
                A("dve", tt(actT[:, f, :], sgb[s4], banks[bsets[1][c]][:, :], ALU.mult),
                  reads=[("sgb", s4), BK(bsets[1][c])], writes=[("actT", f)])
                relbank(bsets[1][c])
        out_proj(ti, w_down[l], NFF, actT, lambda kc: [("actT", kc)], src, srcname, dst, dstname, pre, post)
        Sd.fence()

    stages = []
    for l in range(n_layers):
        for ti in range(NTILE):
            stages.append(("A", l, ti))
        for ti in range(NTILE):
            stages.append(("B", l, ti))

    def stage_io(st):
        kind, l, ti = st
        last = l == n_layers - 1
        if kind == "A":
            src, srcn = (x_in, "x") if l == 0 else (xs, "xs")
            return src, srcn, xs, "xs"
        dst, dstn = (y_out, "y") if last else (xs, "xs")
        return xs, "xs", dst, dstn

    def make_pre(st):
        kind, l, ti = st
        gi = 0 if kind == "A" else 1
        src, srcn, _, _ = stage_io(st)

        def pre(tb):
            if tb == 0 and ti == 0:
                gsrc = g1b_d[l] if kind == "A" else g2b_d[l]
                A("sp", dma(gbuf[gi][:], gsrc), writes=[("gb", gi)], dma=f"p{gi}")
            norm_pre(src, srcn, ti, tb, gbuf[gi], ("gb", gi))
        return pre

    pre0 = make_pre(stages[0])
    for tb in range(4):
        pre0(tb)
        norm_post(tb)
    for k, st in enumerate(stages):
        kind, l, ti = st
        nxt = stages[k + 1] if k + 1 < len(stages) else None
        pre = make_pre(nxt) if nxt is not None else None
        post = norm_post if nxt is not None else None
        src, srcn, dst, dstn = stage_io(st)
        if kind == "A":
            if ti == 0:
                A("sp", dma(pp[:], pp_d[l]), writes=["pp"], dma="p2")
                A("sp", dma(aw[:], aw_d[l]), writes=["aw"], dma="p3")
                A("sp", dma(ab[:], ab_d[l]), writes=["ab"], dma="p4")
                A("pool", dma(pwb[:], pw_d[l].rearrange("g c d -> c g d")), writes=["pwb"], dma="p5")
            sweep_a(l, ti, src, srcn, dst, dstn, pre, post)
        else:
            sweep_b(l, ti, src, srcn, dst, dstn, pre, post)

    fin = A("sp", None, reads=[("y", r, n) for r in range(S // 128) for n in range(4)]
            + [k for k in Sd.lastw if isinstance(k, tuple) and k[0] == "dbg"])

    counters = Sd.finalize()

    sems = {}
    for k in counters:
        nm = "s_" + "_".join(str(v) for v in k)
        sems[k] = es.enter_context(nc.semaphore(nm))
    block = es.enter_context(nc.Block())

    def emit(engname):
        def body(eng):
            for op in Sd.ops[engname]:
                for d in op.waits:
                    eng.wait_ge(sems[d.semkey], d.count)
                if op.fn is None:
                    continue
                ins = op.fn(eng)
                if op.needs_inc:
                    ins.then_inc(sems[op.semkey], 16 if op.dma is not None else 1)
        return body

    block.tensor(emit("pe"))
    block.scalar(emit("act"))
    block.vector(emit("dve"))
    block.gpsimd(emit("pool"))
    block.sync(emit("sp"))
    es.close()
    stats = {e: len(Sd.ops[e]) for e in ENGS}
    return nc, stats


def make_consts():
    c = np.zeros((128, NCST), np.float32)
    i = np.arange(128)
    c[:, K_IDENT:K_IDENT + 128] = np.eye(128, dtype=np.float32)
    c[:, K_UINC:K_UINC + 128] = (i[:, None] >= i[None, :]).astype(np.float32)
    same = (i[:, None] // 64) == (i[None, :] // 64)
    c[:, K_TRI:K_TRI + 128] = ((i[:, None] <= i[None, :]) & same).astype(np.float32)
    c[:, K_SBM:K_SBM + 128] = (i[:, None] < i[None, :]).astype(np.float32)
    tt_ = np.arange(16)
    for g in range(4):
        w = 2 << g
        c[:, K_PRC + g * 16:K_PRC + (g + 1) * 16] = (1.0 / np.minimum(tt_ + 1, w)).astype(np.float32)[None, :]
    return c


def host_layout(inputs):
    f = lambda a: np.ascontiguousarray(np.asarray(a, dtype=np.float32))
    conv_w = f(inputs["conv_w"])
    pp = np.zeros((L, 128, NPP), np.float32)
    cw = conv_w.reshape(L, 3, 4, 128)
    pp[:, :, PP_CONV:PP_CONV + 12] = cw.transpose(0, 3, 2, 1).reshape(L, 128, 12)
    pp[:, :, PP_SBQ] = f(inputs["sb_q_g"])
    pp[:, :, PP_SBK] = f(inputs["sb_k_g"])
    pp[:, :, PP_GLN] = f(inputs["gla_norm_g"])
    pp[:, :, PP_PSC:PP_PSC + 4] = f(inputs["pool_scale"]).reshape(L, 4, 128).transpose(0, 2, 1)
    shared = {
        "w_in": f(inputs["w_in"]),
        "w_out": f(inputs["w_out"]),
        "w_gate": f(inputs["w_gate"]),
        "w_up": f(inputs["w_up"]),
        "w_down": f(inputs["w_down"]),
        "g1b": np.ascontiguousarray(np.broadcast_to(f(inputs["norm1_g"])[:, None, :], (L, 128, D))),
        "g2b": np.ascontiguousarray(np.broadcast_to(f(inputs["norm2_g"])[:, None, :], (L, 128, D))),
        "pp": pp,
        "aw": f(inputs["gla_a_w"]),
        "ab": f(inputs["gla_a_b"]).reshape(L, 1, 256),
        "pool_w": f(inputs["pool_w"]),
        "cst": make_consts(),
    }
    return shared


_CACHE = {}


def kernel(**inputs):
    x = np.ascontiguousarray(np.asarray(inputs["x"], dtype=np.float32))
    shared = host_layout(inputs)
    if "nc" not in _CACHE:
        _CACHE["nc"] = build_program(L)[0]
    nc = _CACHE["nc"]
    in_maps = [dict(shared, x=x[b]) for b in range(NCORES)]
    res = run_bass_kernel_spmd(nc, in_maps, core_ids=list(range(NCORES)))
    out = np.stack([np.asarray(r["y"], dtype=np.float32) for r in res.results], axis=0)
    return out
```

```python
import contextlib
from collections import deque

import numpy as np
import concourse.bass as bass
import concourse.mybir as mybir
from concourse.bass_utils import run_bass_kernel_spmd

F32 = mybir.dt.float32
BF16 = mybir.dt.bfloat16
AF = mybir.ActivationFunctionType
ALU = mybir.AluOpType

D = 2048
S = 2048
L = 4
NCORES = 8
INC = 5136
DFF = 5632
NFF = DFF // 128
TT = 512
NTILE = S // TT
EPS = 1e-6
NW = 4
SQRT128 = float(np.sqrt(128.0))
import os
SAME_ENGINE_SYNC = os.environ.get("K_SES", "1") == "1"

C_CONV = 0
C_SQ = 1536
C_SK = 2048
C_SV = 2560
C_GQ = 3072
C_GK = 3328
C_GV = 3584
C_GR = 4096
C_GA = 4608
C_PU = 4624

PP_CONV = 0
PP_SBQ = 12
PP_SBK = 13
PP_GLN = 14
PP_PSC = 15
NPP = 19

K_IDENT = 0
K_UINC = 128
K_TRI = 256
K_SBM = 384
K_PRC = 384 + 128
NCST = K_PRC + 64


ENGS = ("pe", "act", "dve", "pool", "sp")


class Op:
    __slots__ = ("eng", "fn", "deps", "dma", "needs_inc", "count", "semkey", "idx", "waits")


class Sched:
    def __init__(self):
        self.ops = {e: [] for e in ENGS}
        self.lastw = {}
        self.readers = {}
        self.last_by_key = {}

    def add(self, eng, fn, reads=(), writes=(), dma=None):
        op = Op()
        op.eng = eng
        op.fn = fn
        op.dma = dma
        op.semkey = ("dma", dma) if dma is not None else ("eng", eng)
        op.needs_inc = dma is not None
        op.count = None
        op.waits = None
        deps = set()
        for r in reads:
            w = self.lastw.get(r)
            if w is not None:
                deps.add(w)
        for wkey in writes:
            w = self.lastw.get(wkey)
            if w is not None:
                deps.add(w)
            rd = self.readers.get(wkey)
            if rd:
                deps.update(rd.values())
        op.deps = deps
        for r in reads:
            self.readers.setdefault(r, {})[op.semkey] = op
        for wkey in writes:
            self.lastw[wkey] = op
            self.readers[wkey] = {}
        op.idx = len(self.ops[eng])
        self.ops[eng].append(op)
        if fn is not None and dma is None:
            self.last_by_key[op.semkey] = op
        return op

    def fence(self):
        lasts = [self.last_by_key[("eng", e)] for e in ("pe", "act", "dve") if ("eng", e) in self.last_by_key]
        for e in ("pe", "act", "dve"):
            op = self.add(e, None)
            op.deps = set(lasts)

    def finalize(self):
        for e in ENGS:
            known = {}
            for op in self.ops[e]:
                need = {}
                for d in op.deps:
                    if d.dma is None and d.eng == e:
                        if e == "pe" or not SAME_ENGINE_SYNC:
                            continue
                    k = d.semkey
                    if known.get(k, -1) >= d.idx:
                        continue
                    if k not in need or need[k].idx < d.idx:
                        need[k] = d
                for k, d in need.items():
                    known[k] = d.idx
                    d.needs_inc = True
                op.waits = list(need.values())
        counters = {}
        for e in ENGS:
            for op in self.ops[e]:
                if op.needs_inc and op.fn is not None:
                    k = op.semkey
                    counters[k] = counters.get(k, 0) + (16 if op.dma is not None else 1)
                    op.count = counters[k]
        return counters


def build_program(n_layers=L, dbg=None, dbg_tile=0):
    nc = bass.Bass("TRN2", target_bir_lowering=False)
    dbg = dbg or {}

    def din(name, shape):
        return nc.dram_tensor(name, list(shape), F32, kind="ExternalInput").ap()

    x_in = din("x", (S, D))
    w_in = din("w_in", (L, D, INC))
    w_out = din("w_out", (L, D, D))
    w_gate = din("w_gate", (L, D, DFF))
    w_up = din("w_up", (L, D, DFF))
    w_down = din("w_down", (L, DFF, D))
    g1b_d = din("g1b", (L, 128, D))
    g2b_d = din("g2b", (L, 128, D))
    pp_d = din("pp", (L, 128, NPP))
    aw_d = din("aw", (L, 16, 256))
    ab_d = din("ab", (L, 1, 256))
    pw_d = din("pool_w", (L, 4, 128, 128))
    cst_d = din("cst", (128, NCST))
    y_out = nc.dram_tensor("y", [S, D], F32, kind="ExternalOutput").ap()
    xs = nc.dram_tensor("xs", [S, D], F32, kind="Internal").ap()
    dbg_out = {}
    for name, shape in dbg.items():
        dbg_out[name] = nc.dram_tensor("dbg_" + name, list(shape), F32, kind="ExternalOutput").ap()

    Sd = Sched()
    es = contextlib.ExitStack()

    def sb(name, shape, dt):
        return es.enter_context(nc.sbuf_tensor("sb_" + name, list(shape), dt))

    cst = sb("cst", (128, NCST), F32)
    identb = sb("identb", (128, 128), BF16)
    onesb = sb("onesb", (128, 128), BF16)
    onesf = sb("onesf", (128, 128), F32)
    uincb = sb("uincb", (128, 128), BF16)
    gbuf = [sb(f"gb{i}", (128, D), F32) for i in range(2)]
    pp = sb("pp", (128, NPP), F32)
    aw = sb("aw", (16, 256), F32)
    ab = sb("ab", (1, 256), F32)
    pwb = sb("pwb", (128, 4, 128), BF16)
    hT = sb("hT", (128, 16, TT), BF16)
    wslots = [sb(f"wslot{i}", (128, 16 * 256), BF16) for i in range(NW)]
    xring = [sb(f"xring{i}", (128, D), F32) for i in range(2)]
    hbring = [sb(f"hbring{i}", (128, D), BF16) for i in range(2)]
    stat = sb("stat", (128, 64), F32)
    NXO = 4
    xo = [sb(f"xo{i}", (128, 512), F32) for i in range(NXO)]
    ccar = sb("ccar", (128, 4, 2), F32)
    pcar = sb("pcar", (128, 4, 16), F32)
    dbgbuf = [sb(f"dbgbuf{i}", (128, 512), F32) for i in range(1)] if dbg else None

    UN = 52224
    U = sb("U", (128, UN), BF16)
    ucur = [0]

    def carve(nelem, dt):
        n16 = nelem * (2 if dt == F32 else 1)
        a = ucur[0]
        assert a + n16 <= UN, f"union overflow {a + n16} > {UN}"
        ucur[0] = a + n16
        v = U[:, a:a + n16]
        if dt == F32:
            v = v.bitcast(F32)
        return v

    banks = [es.enter_context(nc.psum_tensor(f"bank{i}", [128, 512], F32)) for i in range(8)]
    freeb = deque(range(8))

    def getbank():
        return freeb.popleft()

    def relbank(b):
        freeb.append(b)

    def BK(b):
        return ("B", b)

    def A(eng, fn, reads=(), writes=(), dma=None):
        return Sd.add(eng, fn, reads, writes, dma)

    def mm(out, lhsT, rhs, start, stop):
        return lambda e: e.matmul(out, lhsT, rhs, start=start, stop=stop)

    def act(out, in_, func, **kw):
        return lambda e: e.activation(out=out, in_=in_, func=func, **kw)

    def tcopy(out, in_):
        return lambda e: e.tensor_copy(out=out, in_=in_)

    def tt(out, in0, in1, op):
        return lambda e: e.tensor_tensor(out=out, in0=in0, in1=in1, op=op)

    def ts(out, in0, s1, s2, op0, op1=None):
        if op1 is None:
            return lambda e: e.tensor_scalar(out=out, in0=in0, scalar1=s1, scalar2=None, op0=op0)
        return lambda e: e.tensor_scalar(out=out, in0=in0, scalar1=s1, scalar2=s2, op0=op0, op1=op1)

    def stt(out, in0, scalar, in1, op0, op1):
        return lambda e: e.scalar_tensor_tensor(out=out, in0=in0, scalar=scalar, in1=in1, op0=op0, op1=op1)

    def dma(out, in_):
        return lambda e: e.dma_start(out=out, in_=in_)

    def memset(ap, v):
        return lambda e: e.memset(ap, v)

    dbg_n = [0]

    def dump(name, src_ap, reads, rows=None):
        if name not in dbg_out:
            return
        dst = dbg_out[name]
        dbg_n[0] += 1
        A("sp", dma(dst if rows is None else dst[rows], src_ap), reads=reads, writes=[("dbg", name, dbg_n[0])],
          dma=f"dbg{dbg_n[0] % 4}")

    stat_i = [0]

    def statcol():
        c = stat_i[0] % 64
        stat_i[0] += 1
        return c

    A("sp", dma(cst[:], cst_d[:, :]), writes=["cst"], dma="c0")
    A("dve", tcopy(identb[:], cst[:, K_IDENT:K_IDENT + 128]), reads=["cst"], writes=["identb"])
    A("dve", tcopy(uincb[:], cst[:, K_UINC:K_UINC + 128]), reads=["cst"], writes=["uincb"])
    A("dve", memset(onesb[:], 1.0), writes=["onesb"])
    A("dve", memset(onesf[:], 1.0), writes=["onesf"])
    A("dve", memset(stat[:], 12345.0), writes=[("stat", c) for c in range(64)])

    wn = [0]

    def wload(src3, nk, ncol):
        s = wn[0] % NW
        wn[0] += 1
        view = wslots[s][:, 0:nk * ncol].rearrange("p (k n) -> p k n", k=nk)
        A("pool", dma(view, src3), writes=[("w", s)], dma=f"w{s}")
        return view, ("w", s)

    def wsrc(wl, r0, nk, c0, ncol):
        return wl[r0:r0 + nk * 128, c0:c0 + ncol].rearrange("(k p) n -> p k n", p=128)

    HT_ALL = [("hT", tb) for tb in range(4)]

    def fm_group(wt, wkey, off, M, nk=16, rhs_of=None, rhs_keys=None):
        b = getbank()
        for kc in range(nk):
            rhs = hT[:, kc, :] if rhs_of is None else rhs_of(kc)
            A("pe", mm(banks[b][0:M, :], wt[:, kc, off:off + M], rhs, kc == 0, kc == nk - 1),
              reads=[wkey] + (HT_ALL if rhs_keys is None else rhs_keys), writes=[BK(b)])
        return b

    def tm_group(wt, wkey, tb, ncol):
        b = getbank()
        for kc in range(16):
            A("pe", mm(banks[b][:, 0:ncol], hT[:, kc, tb * 128:(tb + 1) * 128], wt[:, kc, 0:ncol], kc == 0, kc == 15),
              reads=[wkey, ("hT", tb)], writes=[BK(b)])
        return b

    xn = [0]
    evac_flip = [0]

    norm_slot = {}

    def norm_pre(src, srcname, ti, tb, gb, gbkey):
        r = ti * 4 + tb
        sl = xn[0] % 2
        xn[0] += 1
        norm_slot[tb] = sl
        xk = ("xring", sl)
        hk = ("hbring", sl)
        A("sp", dma(xring[sl][:], src[r * 128:(r + 1) * 128, :]),
          reads=[(srcname, r, n) for n in range(4)], writes=[xk], dma=f"xl{sl}")
        c = statcol()
        A("act", act(hbring[sl][:], xring[sl][:], AF.Square, accum_out=stat[:, c:c + 1]),
          reads=[xk], writes=[hk, ("stat", c)])
        A("act", act(stat[:, c:c + 1], stat[:, c:c + 1], AF.Ln, scale=1.0 / D, bias=EPS),
          reads=[("stat", c)], writes=[("stat", c)])
        A("act", act(stat[:, c:c + 1], stat[:, c:c + 1], AF.Exp, scale=-0.5),
          reads=[("stat", c)], writes=[("stat", c)])
        A("dve", stt(hbring[sl][:], xring[sl][:], stat[:, c:c + 1], gb[:], ALU.mult, ALU.mult),
          reads=[xk, ("stat", c), gbkey], writes=[hk])

    def norm_post(tb):
        sl = norm_slot[tb]
        hk = ("hbring", sl)
        for half in range(2):
            b = getbank()
            bv = banks[b][:].bitcast(BF16)
            for k8 in range(8):
                kc = half * 8 + k8
                A("pe", lambda e, o=bv[:, k8 * 128:(k8 + 1) * 128], i=hbring[sl][:, kc * 128:(kc + 1) * 128]:
                  e.transpose(out=o, in_=i, identity=identb[:]),
                  reads=[hk, "identb"], writes=[BK(b)])
            src_v = bv[:, 0:1024].rearrange("p (k t) -> p k t", k=8)
            dst_v = hT[:, half * 8:(half + 1) * 8, tb * 128:(tb + 1) * 128]
            eng = "act" if (evac_flip[0] % 2 == 0) else "dve"
            evac_flip[0] += 1
            if eng == "act":
                A("act", act(dst_v, src_v, AF.Copy), reads=[BK(b)], writes=[("hT", tb)])
            else:
                A("dve", tcopy(dst_v, src_v), reads=[BK(b)], writes=[("hT", tb)])
            relbank(b)

    xon = [0]

    def out_proj(ti, wl, nkc, actT, actkeys_of, src, srcname, dst, dstname, pre=None, post=None):
        ktiles = [(k0, min(8, nkc - k0)) for k0 in range(0, nkc, 8)]
        for ng in range(4):
            xslots = []
            for tb in range(4):
                r = ti * 4 + tb
                s = xon[0] % NXO
                xon[0] += 1
                xslots.append(s)
                A("sp", dma(xo[s][:], src[r * 128:(r + 1) * 128, ng * 512:(ng + 1) * 512]),
                  reads=[(srcname, r, ng)], writes=[("xo", s)], dma=f"xo{s}")
            if pre is not None:
                pre(ng)
            bs = [getbank() for _ in range(4)]
            for (k0, nk) in ktiles:
                wt, wkey = wload(wsrc(wl, k0 * 128, nk, ng * 512, 512), nk, 512)
                for tb in range(4):
                    for k8 in range(nk):
                        kc = k0 + k8
                        A("pe", mm(banks[bs[tb]][:, :], actT[:, kc, tb * 128:(tb + 1) * 128], wt[:, k8, :],
                                   kc == 0, kc == nkc - 1),
                          reads=[wkey] + actkeys_of(kc), writes=[BK(bs[tb])])
            if post is not None and ng > 0:
                post(ng - 1)
            for tb in range(4):
                r = ti * 4 + tb
                s = xslots[tb]
                A("dve", tt(xo[s][:], xo[s][:], banks[bs[tb]][:, :], ALU.add),
                  reads=[("xo", s), BK(bs[tb])], writes=[("xo", s)])
                A("sp", dma(dst[r * 128:(r + 1) * 128, ng * 512:(ng + 1) * 512], xo[s][:]),
                  reads=[("xo", s)], writes=[(dstname, r, ng)], dma=f"xs{s}")
                relbank(bs[tb])
        if post is not None:
            post(3)

    ucur[0] = 0
    mixedT = carve(16 * TT, BF16).rearrange("p (c t) -> p c t", c=16)
    kTc = carve(4 * S, BF16).rearrange("p (h t) -> p h t", h=4)
    Vc = carve(16 * 512, BF16).rearrange("p (b c) -> p b c", b=16)
    gstate = carve(2 * 256, F32).rearrange("p (c v) -> p c v", c=2)
    gstate_b = carve(2 * 256, BF16).rearrange("p (c v) -> p c v", c=2)
    a_mix_base = ucur[0]
    ubuf = carve(4 * 514, F32).rearrange("p (c t) -> p c t", c=4)
    ybuf = carve(4 * 512, F32).rearrange("p (c t) -> p c t", c=4)
    cbuf = carve(4 * 512, F32).rearrange("p (c t) -> p c t", c=4)
    conv_end = ucur[0]
    ubp = carve(4 * 528, F32).rearrange("p (c t) -> p c t", c=4)
    plev = [carve(528, F32) for _ in range(4)]
    pooled_f = [carve(512, F32) for _ in range(2)]
    pooled_b = [carve(512, BF16) for _ in range(2)]
    pool_end = ucur[0]
    ucur[0] = a_mix_base
    qT = carve(4 * 512, BF16).rearrange("p (h t) -> p h t", h=4)
    qf = [carve(512, F32) for _ in range(2)]
    sqb = [carve(512, BF16) for _ in range(2)]
    rsb = [carve(512, F32) for _ in range(2)]
    NEB = 4
    Eb = [carve(512, F32) for _ in range(NEB)]
    spb = [carve(512, BF16) for _ in range(NEB)]
    cumb = [carve(512, F32) for _ in range(2)]
    Xb = [carve(512, F32) for _ in range(2)]
    Ab = [carve(512, BF16) for _ in range(NEB)]
    totb = carve(512, F32)
    sb_end = ucur[0]
    ucur[0] = a_mix_base
    gqT = carve(2 * 512, F32).rearrange("p (c t) -> p c t", c=2)
    gkT = carve(2 * 512, F32).rearrange("p (c t) -> p c t", c=2)
    gktm = carve(4 * 256, F32).rearrange("p (b c) -> p b c", b=4)
    gv = carve(4 * 512, BF16).rearrange("p (b c) -> p b c", b=4)
    srT = carve(4 * 512, F32).rearrange("p (c t) -> p c t", c=4)
    alr = carve(512, F32)
    oT = carve(4 * 512, F32).rearrange("p (h t) -> p h t", h=4)
    g_e1 = [carve(256, F32) for _ in range(2)]
    g_gp = [carve(256, F32) for _ in range(2)]
    g_eb = [carve(256, F32).rearrange("p (c t) -> p c t", c=2) for _ in range(2)]
    g_enb = [carve(256, F32).rearrange("p (c t) -> p c t", c=2) for _ in range(2)]
    g_enbtm = [carve(256, F32) for _ in range(2)]
    g_qe = [carve(256, BF16).rearrange("p (c t) -> p c t", c=2) for _ in range(2)]
    g_ke = [carve(256, BF16).rearrange("p (c t) -> p c t", c=2) for _ in range(2)]
    g_ketm = [carve(256, BF16) for _ in range(2)]
    g_att = [carve(128, BF16) for _ in range(4)]
    g_sq = [carve(512, BF16) for _ in range(1)]
    g_rn = [carve(512, F32) for _ in range(1)]
    gla_end = ucur[0]
    a_end = max(conv_end, pool_end, sb_end, gla_end)
    ucur[0] = 0
    actT = carve(NFF * TT, BF16).rearrange("p (c t) -> p c t", c=NFF)
    sgb = [carve(512, F32) for _ in range(4)]
    b_end = ucur[0]
    assert max(a_end, b_end) <= UN

    w_tri = cst[:, K_TRI:K_TRI + 128]

    def sweep_a(l, ti, src, srcname, dst, dstname, pre, post):
        t0 = ti * TT
        wl = w_in[l]

        if ti == 0:
            A("dve", memset(ccar[:], 0.0), writes=["ccar"])
        A("dve", tcopy(ubuf[:, :, 0:2], ccar[:]), reads=["ccar"], writes=[("ubuf", c) for c in range(4)])
        for t in (2, 3, 4, 5, 0, 1):
            wt, wkey = wload(wsrc(wl, 0, 16, C_CONV + t * 256, 256), 16, 256)
            for j in range(2):
                c = (t % 2) * 2 + j
                b = fm_group(wt, wkey, j * 128, 128)
                if 2 <= t < 4:
                    A("act", act(cbuf[:, c, :], banks[b][:, :], AF.Copy), reads=[BK(b)], writes=[("cbuf", c)])
                elif t >= 4:
                    yt = ybuf[:, c, :]
                    yk = ("ybuf", c)
                    A("dve", tt(ubuf[:, c, 2:514], cbuf[:, c, :], banks[b][:, :], ALU.mult),
                      reads=[("cbuf", c), BK(b)], writes=[("ubuf", c)])
                    A("dve", ts(yt, ubuf[:, c, 2:514], pp[:, PP_CONV + c * 3 + 2:PP_CONV + c * 3 + 3], None, ALU.mult),
                      reads=[("ubuf", c), "pp"], writes=[yk])
                    A("dve", stt(yt, ubuf[:, c, 1:513], pp[:, PP_CONV + c * 3 + 1:PP_CONV + c * 3 + 2], yt,
                                 ALU.mult, ALU.add), reads=[("ubuf", c), "pp", yk], writes=[yk])
                    A("dve", stt(yt, ubuf[:, c, 0:512], pp[:, PP_CONV + c * 3 + 0:PP_CONV + c * 3 + 1], yt,
                                 ALU.mult, ALU.add), reads=[("ubuf", c), "pp", yk], writes=[yk])
                    A("dve", tcopy(ccar[:, c, :], ubuf[:, c, 512:514]), reads=[("ubuf", c)], writes=["ccar"])
                else:
                    A("dve", tt(mixedT[:, c, :], ybuf[:, c, :], banks[b][:, :], ALU.mult),
                      reads=[("ybuf", c), BK(b)], writes=[("mixedT", c)])
                relbank(b)

        if ti == 0:
            A("dve", memset(pcar[:], 0.0), writes=["pcar"])
        A("dve", tcopy(ubp[:, :, 0:16], pcar[:]), reads=["pcar"], writes=[("ubp", g) for g in range(4)])
        for t in range(2):
            wt, wkey = wload(wsrc(wl, 0, 16, C_PU + t * 256, 256), 16, 256)
            for j in range(2):
                g = t * 2 + j
                win = 2 << g
                b = fm_group(wt, wkey, j * 128, 128)
                A("act", act(ubp[:, g, 16:528], banks[b][:, :], AF.Copy), reads=[BK(b)], writes=[("ubp", g)])
                relbank(b)
        for t in range(2):
            for j in range(2):
                g = t * 2 + j
                win = 2 << g
                prev = ubp[:, g, :]
                prevk = ("ubp", g)
                sh = 1
                lo = 1
                for lev in range(g + 1):
                    cur = plev[lev]
                    A("dve", tt(cur[:, lo:528], prev[:, lo:528], prev[:, lo - sh:528 - sh], ALU.add),
                      reads=[prevk], writes=[("plev", lev)])
                    prev = cur
                    prevk = ("plev", lev)
                    sh *= 2
                    lo += sh
                pf = pooled_f[g % 2]
                pb_ = pooled_b[g % 2]
                A("dve", stt(pf, prev[:, 16:528], 1.0 / win, ubp[:, g, 16:528], ALU.mult, ALU.subtract),
                  reads=[prevk, ("ubp", g)], writes=[("pooled_f", g % 2)])
                if ti == 0:
                    A("dve", tt(pf[:, 0:16], prev[:, 16:32], cst[:, K_PRC + g * 16:K_PRC + (g + 1) * 16], ALU.mult),
                      reads=[prevk, "cst", ("pooled_f", g % 2)], writes=[("pooled_f", g % 2)])
                    A("dve", tt(pf[:, 0:16], pf[:, 0:16], ubp[:, g, 16:32], ALU.subtract),
                      reads=[("ubp", g), ("pooled_f", g % 2)], writes=[("pooled_f", g % 2)])
                A("dve", tcopy(pb_, pf), reads=[("pooled_f", g % 2)], writes=[("pooled_b", g % 2)])
                A("dve", tcopy(pcar[:, g, :], ubp[:, g, 512:528]), reads=[("ubp", g)], writes=["pcar"])
                b2 = getbank()
                A("pe", mm(banks[b2][:, :], pwb[:, g, :], pb_, True, True),
                  reads=["pwb", ("pooled_b", g % 2)], writes=[BK(b2)])
                A("act", act(mixedT[:, 12 + g, :], banks[b2][:, :], AF.Copy, scale=pp[:, PP_PSC + g:PP_PSC + g + 1]),
                  reads=[BK(b2), "pp"], writes=[("mixedT", 12 + g)])
                relbank(b2)
        Sd.fence()

        qi = 0
        pending = [None]

        def sb_norm_chain(s2, isq, h):
            def run():
                b2 = getbank()
                A("pe", mm(banks[b2][:, :], onesb[:], sqb[s2], True, True), reads=["onesb", ("sqb", s2)], writes=[BK(b2)])
                A("act", act(rsb[s2], banks[b2][:, :], AF.Ln, bias=128.0 * EPS), reads=[BK(b2)], writes=[("rsb", s2)])
                relbank(b2)
                A("act", act(rsb[s2], rsb[s2], AF.Exp, scale=-0.5), reads=[("rsb", s2)], writes=[("rsb", s2)])
                gcol = PP_SBQ if isq else PP_SBK
                if isq:
                    dstv = qT[:, h, :]
                    dk = ("qT", h)
                else:
                    dstv = kTc[:, h, t0:t0 + TT]
                    dk = ("kT", h, ti)
                A("dve", stt(dstv, qf[s2], pp[:, gcol:gcol + 1], rsb[s2], ALU.mult, ALU.mult),
                  reads=[("qf", s2), "pp", ("rsb", s2)], writes=[dk])
            return run

        for t in range(4):
            wt, wkey = wload(wsrc(wl, 0, 16, C_SQ + t * 256, 256), 16, 256)
            for j in range(2):
                h = (t % 2) * 2 + j
                isq = t < 2
                b = fm_group(wt, wkey, j * 128, 128)
                s2 = qi % 2
                qi += 1
                A("act", act(qf[s2], banks[b][:, :], AF.Copy), reads=[BK(b)], writes=[("qf", s2)])
                A("act", act(sqb[s2], banks[b][:, :], AF.Square), reads=[BK(b)], writes=[("sqb", s2)])
                relbank(b)
                if pending[0] is not None:
                    pending[0]()
                pending[0] = sb_norm_chain(s2, isq, h)
        sv_first = True
        for t in range(2):
            wt, wkey = wload(wsrc(wl, 0, 16, C_SV + t * 256, 256), 16, 256)
            for tb in range(4):
                b = tm_group(wt, wkey, tb, 256)
                A("act", act(Vc[:, ti * 4 + tb, t * 256:(t + 1) * 256], banks[b][:, 0:256], AF.Copy),
                  reads=[BK(b)], writes=[("Vc", ti * 4 + tb, t)])
                relbank(b)
                if pending[0] is not None:
                    pending[0]()
                    pending[0] = None
        pairs = [(h, kb) for h in range(4) for kb in range(ti * 4 + 3, -1, -1)]
        NP = len(pairs)
        st = {}
        po_bank = {}

        def stage_a(n):
            h, kb = pairs[n]
            j = kb - ti * 4
            q0 = max(j, 0) * 128
            e = n % NEB
            bz = getbank()
            A("pe", mm(banks[bz][:, q0:512], kTc[:, h, kb * 128:(kb + 1) * 128], qT[:, h, q0:512], True, True),
              reads=[("kT", h, kb // 4), ("qT", h)], writes=[BK(bz)])
            A("act", act(Eb[e][:, q0:512], banks[bz][:, q0:512], AF.Exp, scale=SQRT128), reads=[BK(bz)], writes=[("E", e)])
            relbank(bz)
            if j >= 0:
                A("dve", tt(Eb[e][:, q0:q0 + 128], Eb[e][:, q0:q0 + 128], cst[:, K_SBM:K_SBM + 128], ALU.mult),
                  reads=[("E", e), "cst"], writes=[("E", e)])
            A("act", act(spb[e][:, q0:512], Eb[e][:, q0:512], AF.Ln, bias=1.0), reads=[("E", e)], writes=[("sp", e)])

        def stage_b(n):
            h, kb = pairs[n]
            j = kb - ti * 4
            q0 = max(j, 0) * 128
            e = n % NEB
            first = kb == ti * 4 + 3
            last = kb == 0
            if first:
                A("dve", memset(totb, 0.0), writes=["tot"])
            bc = getbank()
            A("pe", mm(banks[bc][:, q0:512], uincb[:], spb[e][:, q0:512], True, True), reads=["uincb", ("sp", e)], writes=[BK(bc)])
            c2 = n % 2
            A("dve", tt(cumb[c2][:, q0:512], banks[bc][:, q0:512], totb[:, q0:512], ALU.add),
              reads=[BK(bc), "tot"], writes=[("cum", c2)])
            relbank(bc)
            if not last:
                bo = getbank()
                A("pe", mm(banks[bo][:, q0:512], onesb[:], spb[e][:, q0:512], True, True), reads=["onesb", ("sp", e)], writes=[BK(bo)])
                A("dve", tt(totb[:, q0:512], totb[:, q0:512], banks[bo][:, q0:512], ALU.add), reads=[BK(bo), "tot"], writes=["tot"])
                relbank(bo)
            A("act", act(Xb[c2][:, q0:512], cumb[c2][:, q0:512], AF.Exp, scale=-1.0), reads=[("cum", c2)], writes=[("X", c2)])
            if q0 > 0:
                A("pool", memset(Ab[e][:, 0:q0], 0.0), writes=[("A", e)])
            A("pool", tt(Ab[e][:, q0:512], Eb[e][:, q0:512], Xb[c2][:, q0:512], ALU.mult), reads=[("E", e), ("X", c2)], writes=[("A", e)])

        def stage_c(n):
            h, kb = pairs[n]
            e = n % NEB
            first = kb == ti * 4 + 3
            last = kb == 0
            if first:
                po_bank[h] = getbank()
            bp = po_bank[h]
            A("pe", mm(banks[bp][:, :], Vc[:, kb, h * 128:(h + 1) * 128], Ab[e], first, last),
              reads=[("Vc", kb, h // 2), ("A", e)], writes=[BK(bp)])
            if last:
                A("act", act(mixedT[:, 4 + h, :], banks[bp][:, :], AF.Copy), reads=[BK(bp)], writes=[("mixedT", 4 + h)])
                relbank(bp)

        gla_pref = [wload(wsrc(wl, 0, 16, C_GQ, 256), 16, 256),
                    wload(wsrc(wl, 0, 16, C_GK, 256), 16, 256),
                    wload(wsrc(wl, 0, 16, C_GV, 256), 16, 256)]
        for n in range(NP + 3):
            if n < NP:
                stage_a(n)
            if 0 <= n - 1 < NP:
                stage_b(n - 1)
            if 0 <= n - 3 < NP:
                stage_c(n - 3)
        Sd.fence()

        if ti == 0:
            A("dve", memset(gstate[:], 0.0), writes=[("gstate", c) for c in range(2)])
            A("dve", memset(gstate_b[:], 0.0), writes=[("gstate_b", c) for c in range(2)])
        wt, wkey = gla_pref[0]
        for c in range(2):
            b = fm_group(wt, wkey, c * 128, 128)
            A("act", act(gqT[:, c, :], banks[b][:, :], AF.Copy), reads=[BK(b)], writes=[("gqT", c)])
            relbank(b)
        wt, wkey = gla_pref[1]
        for c in range(2):
            b = fm_group(wt, wkey, c * 128, 128)
            A("act", act(gkT[:, c, :], banks[b][:, :], AF.Copy), reads=[BK(b)], writes=[("gkT", c)])
            relbank(b)
        for tb in range(4):
            b = tm_group(wt, wkey, tb, 256)
            A("act", act(gktm[:, tb, :], banks[b][:, 0:256], AF.Copy), reads=[BK(b)], writes=[("gktm", tb)])
            relbank(b)
        for t in range(2):
            wt, wkey = gla_pref[2] if t == 0 else wload(wsrc(wl, 0, 16, C_GV + t * 256, 256), 16, 256)
            for tb in range(4):
                b = tm_group(wt, wkey, tb, 256)
                A("act", act(gv[:, tb, t * 256:(t + 1) * 256], banks[b][:, 0:256], AF.Copy),
                  reads=[BK(b)], writes=[("gv", tb, t)])
                relbank(b)
        for t in range(2):
            wt, wkey = wload(wsrc(wl, 0, 16, C_GR + t * 256, 256), 16, 256)
            for j in range(2):
                c = t * 2 + j
                b = fm_group(wt, wkey, j * 128, 128)
                A("act", act(srT[:, c, :], banks[b][:, :], AF.Silu), reads=[BK(b)], writes=[("srT", c)])
                relbank(b)
        wt, wkey = wload(wsrc(wl, 0, 16, C_GA, 16), 16, 16)
        b = fm_group(wt, wkey, 0, 16)
        A("act", act(alr[0:16, :], banks[b][0:16, :], AF.Copy), reads=[BK(b)], writes=["alr"])
        relbank(b)

        def gla_G(tb):
            tsl = slice(tb * 128, (tb + 1) * 128)
            s2 = tb % 2
            e1, gp, eb, enb, enbtm = g_e1[s2], g_gp[s2], g_eb[s2], g_enb[s2], g_enbtm[s2]
            bg = getbank()
            A("pe", mm(banks[bg][:, 0:256], alr[0:16, tsl], aw[0:16, :], True, False), reads=["alr", "aw"], writes=[BK(bg)])
            A("pe", mm(banks[bg][:, 0:256], onesf[0:1, 0:128], ab[0:1, :], False, True), reads=["onesf", "ab"], writes=[BK(bg)])
            A("act", act(e1, banks[bg][:, 0:256], AF.Exp, scale=-1.0), reads=[BK(bg)], writes=[("g_e1", s2)])
            relbank(bg)
            A("act", act(gp, e1, AF.Ln, bias=1.0), reads=[("g_e1", s2)], writes=[("g_gp", s2)])
            bfm = getbank()
            for c in range(2):
                A("pe", mm(banks[bfm][:, c * 128:(c + 1) * 128], gp[:, c * 128:(c + 1) * 128], w_tri, True, True),
                  reads=[("g_gp", s2), "cst"], writes=[BK(bfm)])
            btm = getbank()
            A("pe", mm(banks[btm][:, 0:256], w_tri, gp, True, True), reads=[("g_gp", s2), "cst"], writes=[BK(btm)])
            pfm = banks[bfm][:, 0:256].rearrange("p (c t) -> p c t", c=2)
            A("act", act(eb, pfm, AF.Exp, scale=-1.0 / 16.0), reads=[BK(bfm)], writes=[("g_eb", s2)])
            A("act", act(enb, pfm, AF.Exp, scale=1.0 / 16.0), reads=[BK(bfm)], writes=[("g_enb", s2)])
            relbank(bfm)
            A("act", act(enbtm, banks[btm][:, 0:256], AF.Exp, scale=1.0 / 16.0), reads=[BK(btm)], writes=[("g_enbtm", s2)])
            relbank(btm)
            qe = g_qe[s2]
            ke = g_ke[s2]
            ketm = g_ketm[s2]
            A("dve", stt(qe, gqT[:, :, tsl], 0.125, eb, ALU.mult, ALU.mult),
              reads=[("gqT", 0), ("gqT", 1), ("g_eb", s2)], writes=[("g_qe", s2)])
            A("dve", tt(ke, gkT[:, :, tsl], enb, ALU.mult), reads=[("gkT", 0), ("gkT", 1), ("g_enb", s2)], writes=[("g_ke", s2)])
            A("dve", tt(ketm, gktm[:, tb, :], enbtm, ALU.mult), reads=[("gktm", tb), ("g_enbtm", s2)], writes=[("g_ketm", s2)])

        def gla_R(tb):
            tsl = slice(tb * 128, (tb + 1) * 128)
            s2 = tb % 2
            eb = g_eb[s2]
            qe = g_qe[s2]
            ke = g_ke[s2]
            ketm = g_ketm[s2]
            po_b = {}
            for h in range(4):
                c = h // 2
                p0 = (h % 2) * 64
                ba = getbank()
                A("pe", mm(banks[ba][:, 0:128], ke[p0:p0 + 64, c, :], qe[p0:p0 + 64, c, :], True, True),
                  reads=[("g_ke", s2), ("g_qe", s2)], writes=[BK(ba)])
                a3 = h
                A("dve", tt(g_att[a3], banks[ba][:, 0:128], w_tri, ALU.mult), reads=[BK(ba), "cst"], writes=[("g_att", a3)])
                relbank(ba)
                po_b[h] = (getbank(), a3)
            for ch in range(2):
                csl = slice(ch * 64, (ch + 1) * 64)
                for h in range(4):
                    c = h // 2
                    p0 = (h % 2) * 64
                    bo, a3 = po_b[h]
                    A("pe", lambda e, o=banks[bo][:, ch * 64:(ch + 1) * 64],
                      l_=gstate_b[p0:p0 + 64, c, (h % 2) * 128:(h % 2 + 1) * 128], r_=qe[p0:p0 + 64, c, csl], st_=(ch == 0):
                      e.matmul(o, l_, r_, start=st_, stop=False, skip_group_check=True),
                      reads=[("gstate_b", c), ("g_qe", s2)], writes=[BK(bo)])
                for c in range(2):
                    bs_ = getbank()
                    A("pe", mm(banks[bs_][:, 0:256], ketm[csl, c * 128:(c + 1) * 128], gv[csl, tb, c * 256:(c + 1) * 256],
                               True, True),
                      reads=[("g_ketm", s2), ("gv", tb, c)], writes=[BK(bs_)])
                    A("dve", tt(gstate[:, c, :], gstate[:, c, :], banks[bs_][:, 0:256], ALU.add),
                      reads=[("gstate", c), BK(bs_)], writes=[("gstate", c)])
                    relbank(bs_)
                    A("dve", ts(gstate[:, c, :], gstate[:, c, :], eb[:, c, ch * 64 + 63:ch * 64 + 64], None, ALU.mult),
                      reads=[("gstate", c), ("g_eb", s2)], writes=[("gstate", c)])
                    A("dve", tcopy(gstate_b[:, c, :], gstate[:, c, :]), reads=[("gstate", c)], writes=[("gstate_b", c)])
            for h in range(4):
                bo, a3 = po_b[h]
                A("pe", mm(banks[bo][:, 0:128], gv[:, tb, h * 128:(h + 1) * 128], g_att[a3], False, True),
                  reads=[("gv", tb, h // 2), ("g_att", a3)], writes=[BK(bo)])
                A("act", act(oT[:, h, tsl], banks[bo][:, 0:128], AF.Copy), reads=[BK(bo)], writes=[("oT", h)])
                relbank(bo)

        gla_G(0)
        gla_G(1)
        gla_R(0)
        gla_G(2)
        gla_R(1)
        gla_G(3)
        gla_R(2)
        gla_R(3)
        for h in range(4):
            s2 = 0
            A("act", act(g_sq[s2], oT[:, h, :], AF.Square), reads=[("oT", h)], writes=[("g_sq", s2)])
            bn = getbank()
            A("pe", mm(banks[bn][:, :], onesb[:], g_sq[s2], True, True), reads=["onesb", ("g_sq", s2)], writes=[BK(bn)])
            A("act", act(g_rn[s2], banks[bn][:, :], AF.Ln, bias=128.0 * EPS), reads=[BK(bn)], writes=[("g_rn", s2)])
            relbank(bn)
            A("act", act(g_rn[s2], g_rn[s2], AF.Exp, scale=-0.5), reads=[("g_rn", s2)], writes=[("g_rn", s2)])
            A("dve", stt(oT[:, h, :], oT[:, h, :], pp[:, PP_GLN:PP_GLN + 1], g_rn[s2], ALU.mult, ALU.mult),
              reads=[("oT", h), "pp", ("g_rn", s2)], writes=[("oT", h)])
            A("dve", stt(mixedT[:, 8 + h, :], oT[:, h, :], SQRT128, srT[:, h, :], ALU.mult, ALU.mult),
              reads=[("oT", h), ("srT", h)], writes=[("mixedT", 8 + h)])
        Sd.fence()

        if "mixedT" in dbg_out and ti == dbg_tile and l == 0:
            for c in range(16):
                s = 0
                A("dve", tcopy(dbgbuf[s][:], mixedT[:, c, :]), reads=[("mixedT", c)], writes=[("dbgbuf", s)])
                dump("mixedT", dbgbuf[s][:], [("dbgbuf", s)], rows=(slice(c * 128, (c + 1) * 128), slice(None)))
            Sd.fence()

        out_proj(ti, w_out[l], 16, mixedT, lambda kc: [("mixedT", kc)], src, srcname, dst, dstname, pre, post)
        Sd.fence()

    def sweep_b(l, ti, src, srcname, dst, dstname, pre, post):
        si = 0
        for grp in range(NFF // 4):
            bsets = []
            for wmat in (w_gate[l], w_up[l]):
                bset = [getbank() for _ in range(4)]
                bsets.append(bset)
                for kh in range(2):
                    wt, wk = wload(wsrc(wmat, kh * 1024, 8, grp * 512, 512), 8, 512)
                    for c in range(4):
                        for k8 in range(8):
                            kc = kh * 8 + k8
                            A("pe", mm(banks[bset[c]][:, :], wt[:, k8, c * 128:(c + 1) * 128], hT[:, kc, :],
                                       kc == 0, kc == 15),
                              reads=[wk] + HT_ALL, writes=[BK(bset[c])])
            slots = []
            for c in range(4):
                s4 = si % 4
                si += 1
                slots.append(s4)
                A("act", act(sgb[s4], banks[bsets[0][c]][:, :], AF.Silu), reads=[BK(bsets[0][c])], writes=[("sgb", s4)])
                relbank(bsets[0][c])
            for c in range(4):
                f = grp * 4 + c
                s4 = slots[c]
                A("dve", tt(actT[:, f, :], sgb[s4], banks[bsets[1][c]][:, :], ALU.mult),
                  reads=[("sgb", s4), BK(bsets[1][c])], writes=[("actT", f)])
                relbank(bsets[1][c])
        out_proj(ti, w_down[l], NFF, actT, lambda kc: [("actT", kc)], src, srcname, dst, dstname, pre, post)
        Sd.fence()

    stages = []
    for l in range(n_layers):
        for ti in range(NTILE):
            stages.append(("A", l, ti))
        for ti in range(NTILE):
            stages.append(("B", l, ti))

    def stage_io(st):
        kind, l, ti = st
        last = l == n_layers - 1
        if kind == "A":
            src, srcn = (x_in, "x") if l == 0 else (xs, "xs")
            return src, srcn, xs, "xs"
        dst, dstn = (y_out, "y") if last else (xs, "xs")
        return xs, "xs", dst, dstn

    def make_pre(st):
        kind, l, ti = st
        gi = 0 if kind == "A" else 1
        src, srcn, _, _ = stage_io(st)

        def pre(tb):
            if tb == 0 and ti == 0:
                gsrc = g1b_d[l] if kind == "A" else g2b_d[l]
                A("sp", dma(gbuf[gi][:], gsrc), writes=[("gb", gi)], dma=f"p{gi}")
            norm_pre(src, srcn, ti, tb, gbuf[gi], ("gb", gi))
        return pre

    pre0 = make_pre(stages[0])
    for tb in range(4):
        pre0(tb)
        norm_post(tb)
    for k, st in enumerate(stages):
        kind, l, ti = st
        nxt = stages[k + 1] if k + 1 < len(stages) else None
        pre = make_pre(nxt) if nxt is not None else None
        post = norm_post if nxt is not None else None
        src, srcn, dst, dstn = stage_io(st)
        if kind == "A":
            if ti == 0:
                A("sp", dma(pp[:], pp_d[l]), writes=["pp"], dma="p2")
                A("sp", dma(aw[:], aw_d[l]), writes=["aw"], dma="p3")
                A("sp", dma(ab[:], ab_d[l]), writes=["ab"], dma="p4")
                A("pool", dma(pwb[:], pw_d[l].rearrange("g c d -> c g d")), writes=["pwb"], dma="p5")
            sweep_a(l, ti, src, srcn, dst, dstn, pre, post)
        else:
            sweep_b(l, ti, src, srcn, dst, dstn, pre, post)

    fin = A("sp", None, reads=[("y", r, n) for r in range(S // 128) for n in range(4)]
            + [k for k in Sd.lastw if isinstance(k, tuple) and k[0] == "dbg"])

    counters = Sd.finalize()

    sems = {}
    for k in counters:
        nm = "s_" + "_".join(str(v) for v in k)
        sems[k] = es.enter_context(nc.semaphore(nm))
    block = es.enter_context(nc.Block())

    def emit(engname):
        def body(eng):
            for op in Sd.ops[engname]:
                for d in op.waits:
                    eng.wait_ge(sems[d.semkey], d.count)
                if op.fn is None:
                    continue
                ins = op.fn(eng)
                if op.needs_inc:
                    ins.then_inc(sems[op.semkey], 16 if op.dma is not None else 1)
        return body

    block.tensor(emit("pe"))
    block.scalar(emit("act"))
    block.vector(emit("dve"))
    block.gpsimd(emit("pool"))
    block.sync(emit("sp"))
    es.close()
    stats = {e: len(Sd.ops[e]) for e in ENGS}
    return nc, stats


def make_consts():
    c = np.zeros((128, NCST), np.float32)
    i = np.arange(128)
    c[:, K_IDENT:K_IDENT + 128] = np.eye(128, dtype=np.float32)
    c[:, K_UINC:K_UINC + 128] = (i[:, None] >= i[None, :]).astype(np.float32)
    same = (i[:, None] // 64) == (i[None, :] // 64)
    c[:, K_TRI:K_TRI + 128] = ((i[:, None] <= i[None, :]) & same).astype(np.float32)
    c[:, K_SBM:K_SBM + 128] = (i[:, None] < i[None, :]).astype(np.float32)
    tt_ = np.arange(16)
    for g in range(4):
        w = 2 << g
        c[:, K_PRC + g * 16:K_PRC + (g + 1) * 16] = (1.0 / np.minimum(tt_ + 1, w)).astype(np.float32)[None, :]
    return c


def host_layout(inputs):
    f = lambda a: np.ascontiguousarray(np.asarray(a, dtype=np.float32))
    conv_w = f(inputs["conv_w"])
    pp = np.zeros((L, 128, NPP), np.float32)
    cw = conv_w.reshape(L, 3, 4, 128)
    pp[:, :, PP_CONV:PP_CONV + 12] = cw.transpose(0, 3, 2, 1).reshape(L, 128, 12)
    pp[:, :, PP_SBQ] = f(inputs["sb_q_g"])
    pp[:, :, PP_SBK] = f(inputs["sb_k_g"])
    pp[:, :, PP_GLN] = f(inputs["gla_norm_g"])
    pp[:, :, PP_PSC:PP_PSC + 4] = f(inputs["pool_scale"]).reshape(L, 4, 128).transpose(0, 2, 1)
    shared = {
        "w_in": f(inputs["w_in"]),
        "w_out": f(inputs["w_out"]),
        "w_gate": f(inputs["w_gate"]),
        "w_up": f(inputs["w_up"]),
        "w_down": f(inputs["w_down"]),
        "g1b": np.ascontiguousarray(np.broadcast_to(f(inputs["norm1_g"])[:, None, :], (L, 128, D))),
        "g2b": np.ascontiguousarray(np.broadcast_to(f(inputs["norm2_g"])[:, None, :], (L, 128, D))),
        "pp": pp,
        "aw": f(inputs["gla_a_w"]),
        "ab": f(inputs["gla_a_b"]).reshape(L, 1, 256),
        "pool_w": f(inputs["pool_w"]),
        "cst": make_consts(),
    }
    return shared


_CACHE = {}


def kernel(**inputs):
    x = np.ascontiguousarray(np.asarray(inputs["x"], dtype=np.float32))
    shared = host_layout(inputs)
    if "nc" not in _CACHE:
        _CACHE["nc"] = build_program(L)[0]
    nc = _CACHE["nc"]
    in_maps = [dict(shared, x=x[b]) for b in range(NCORES)]
    res = run_bass_kernel_spmd(nc, in_maps, core_ids=list(range(NCORES)))
    out = np.stack([np.asarray(r["y"], dtype=np.float32) for r in res.results], axis=0)
    return out
```
